# Optimizing a Trainium2 kernel written in Bass

```python
import math
import jax
import jax.numpy as jnp
from jax import lax
import numpy as np

D_MODEL = 1024
BATCH = 2
SEQ = 8192
DEPTH = 4

GRID_W = 64
CTX_LEN = 256
Q_BLOCK = 128
ROPE_THETA = 10000.0
NORM_EPS = 1e-6
N_MOD = 6

A_HEADS = 8
A_KV_HEADS = 2
A_DIM = 64
B_HEADS = 8
B_Q_RANK = 256
B_KV_RANK = 128
B_NOPE = 64
B_ROPE = 32
B_V = 64
MLA_SCALE = (B_NOPE + B_ROPE) ** -0.5
C_HEADS = 8
C_DIM = 32
C_V = 64
N_EXPERTS = 32
TOP_K = 4
D_EXPERT = D_MODEL
SWIGLU_ALPHA = 1.702
SWIGLU_LIMIT = 7.0
E_BLOCK = 128

A_WIDTH = A_HEADS * A_DIM
B_WIDTH = B_HEADS * B_V
C_WIDTH = C_HEADS * C_V
IN_SIZES = (A_HEADS * A_DIM, A_KV_HEADS * A_DIM, A_KV_HEADS * A_DIM,
            B_Q_RANK, B_KV_RANK, B_ROPE,
            C_HEADS * 2 * C_DIM, C_HEADS * 2 * C_DIM, C_HEADS * C_V,
            3 * D_MODEL)
IN_COLS = sum(IN_SIZES)
IN_SPLITS = tuple(int(s) for s in np.cumsum(IN_SIZES)[:-1])

kernel_name = 'hybrid_gqa_mla_diffattn_moe_dit_trunk'


def rms_norm(x, gain):
    xf = x.astype(jnp.float32)
    y = xf * lax.rsqrt(jnp.mean(xf * xf, axis=-1, keepdims=True) + NORM_EPS)
    return (y * gain.astype(jnp.float32)).astype(x.dtype)


def modulate(h, shift, scale):
    return h * (1.0 + scale) + shift


def adaln(cond, w_mod, b_mod):
    return jnp.split(jax.nn.silu(cond) @ w_mod + b_mod, N_MOD, axis=-1)


def axial_rope(n_lat, rot_dim):
    n_rows = n_lat // GRID_W
    row = jnp.repeat(jnp.arange(n_rows, dtype=jnp.float32), GRID_W)
    col = jnp.tile(jnp.arange(GRID_W, dtype=jnp.float32), n_rows)
    axis_pairs = rot_dim // 4
    inv_freq = ROPE_THETA ** (-jnp.arange(axis_pairs, dtype=jnp.float32) / axis_pairs)
    ang = jnp.concatenate([row[:, None] * inv_freq, col[:, None] * inv_freq], axis=-1)
    return jnp.cos(ang), jnp.sin(ang)


def apply_rope(x, cos, sin):
    shape = (1, cos.shape[0]) + (1,) * (x.ndim - 3) + (cos.shape[1],)
    cos = cos.reshape(shape)
    sin = sin.reshape(shape)
    x1, x2 = jnp.split(x.astype(jnp.float32), 2, axis=-1)
    return jnp.concatenate([x1 * cos - x2 * sin, x2 * cos + x1 * sin], axis=-1).astype(x.dtype)


def over_query_blocks(fn, *qs):
    bsz, n = qs[0].shape[:2]
    nb = n // Q_BLOCK
    blocks = tuple(jnp.moveaxis(q.reshape((bsz, nb, Q_BLOCK) + q.shape[2:]), 1, 0) for q in qs)
    out = lax.map(lambda args: fn(*args), blocks)
    return jnp.moveaxis(out, 0, 1).reshape((bsz, n) + out.shape[3:])


def apply_direct(fn, *qs):
    return fn(*qs)


def gqa_attend(q, k, v):
    b, nq, h, d = q.shape
    g = k.shape[2]
    qg = q.reshape(b, nq, g, h // g, d)
    s = jnp.einsum('bqgrd,bkgd->bgrqk', qg, k).astype(jnp.float32) * (d ** -0.5)
    p = jax.nn.softmax(s, axis=-1).astype(v.dtype)
    o = jnp.einsum('bgrqk,bkgd->bqgrd', p, v)
    return o.reshape(b, nq, h, v.shape[-1])


def mla_attend(q_nope, q_rope, k_nope, k_rope, v):
    s = (jnp.einsum('bqhd,bkhd->bhqk', q_nope, k_nope)
         + jnp.einsum('bqhd,bkd->bhqk', q_rope, k_rope)).astype(jnp.float32) * MLA_SCALE
    p = jax.nn.softmax(s, axis=-1).astype(v.dtype)
    return jnp.einsum('bhqk,bkhd->bqhd', p, v)


def diff_attend(q, k, v, lam):
    s = jnp.einsum('bqhcd,bkhcd->bhcqk', q, k).astype(jnp.float32) * (q.shape[-1] ** -0.5)
    p = jax.nn.softmax(s, axis=-1)
    w = (p[:, :, 0] - lam * p[:, :, 1]).astype(v.dtype)
    return jnp.einsum('bhqk,bkhd->bqhd', w, v)


def mixer_inputs(h, w_in, a_q_norm, a_k_norm, b_q_a_norm, b_kv_a_norm, b_w_uq, b_w_ukv,
                 b_q_norm, b_k_norm, c_q_norm, c_k_norm, rope_a, rope_b, rope_c):
    bsz, n, _ = h.shape
    (a_q, a_k, a_v, b_cq, b_ckv, b_kr, c_q, c_k, c_v, gates) = jnp.split(h @ w_in, IN_SPLITS, axis=-1)
    a_q = rms_norm(a_q.reshape(bsz, n, A_HEADS, A_DIM), a_q_norm)
    a_k = rms_norm(a_k.reshape(bsz, n, A_KV_HEADS, A_DIM), a_k_norm)
    a_v = a_v.reshape(bsz, n, A_KV_HEADS, A_DIM)
    b_q = (rms_norm(b_cq, b_q_a_norm) @ b_w_uq).reshape(bsz, n, B_HEADS, B_NOPE + B_ROPE)
    b_kv = (rms_norm(b_ckv, b_kv_a_norm) @ b_w_ukv).reshape(bsz, n, B_HEADS, B_NOPE + B_V)
    b_qn = rms_norm(b_q[..., :B_NOPE], b_q_norm[:B_NOPE])
    b_qr = rms_norm(b_q[..., B_NOPE:], b_q_norm[B_NOPE:])
    b_kn = rms_norm(b_kv[..., :B_NOPE], b_k_norm[:B_NOPE])
    b_v = b_kv[..., B_NOPE:]
    b_kr = rms_norm(b_kr, b_k_norm[B_NOPE:])
    c_q = rms_norm(c_q.reshape(bsz, n, C_HEADS, 2, C_DIM), c_q_norm)
    c_k = rms_norm(c_k.reshape(bsz, n, C_HEADS, 2, C_DIM), c_k_norm)
    c_v = c_v.reshape(bsz, n, C_HEADS, C_V)
    if rope_a is not None:
        a_q = apply_rope(a_q, *rope_a)
        a_k = apply_rope(a_k, *rope_a)
        b_qr = apply_rope(b_qr, *rope_b)
        b_kr = apply_rope(b_kr, *rope_b)
        c_q = apply_rope(c_q, *rope_c)
        c_k = apply_rope(c_k, *rope_c)
    return (a_q, b_qn, b_qr, c_q), (a_k, a_v, b_kn, b_kr, b_v, c_k, c_v), gates


def token_mixers(queries, keys_values, lam, blocked):
    a_q, b_qn, b_qr, c_q = queries
    a_k, a_v, b_kn, b_kr, b_v, c_k, c_v = keys_values
    run = over_query_blocks if blocked else apply_direct
    o_a = run(lambda q: gqa_attend(q, a_k, a_v), a_q)
    o_b = run(lambda qn, qr: mla_attend(qn, qr, b_kn, b_kr, b_v), b_qn, b_qr)
    o_c = run(lambda q: diff_attend(q, c_k, c_v, lam), c_q)
    return o_a, o_b, o_c


def merge_branches(o_a, o_b, o_c, gates, c_subln, lam_init, w_o_a, w_o_b, w_o_c, w_out):
    bsz, n = gates.shape[:2]
    o_c = rms_norm(o_c, c_subln) * (1.0 - lam_init)
    g_a, g_b, g_c = jnp.split(jax.nn.sigmoid(gates), 3, axis=-1)
    y = (g_a * (o_a.reshape(bsz, n, A_WIDTH) @ w_o_a)
         + g_b * (o_b.reshape(bsz, n, B_WIDTH) @ w_o_b)
         + g_c * (o_c.reshape(bsz, n, C_WIDTH) @ w_o_c))
    return y @ w_out


def moe_ffn(h, router_w, router_b, w_gu, b_gu, w_down, b_down):
    bsz, n, d = h.shape
    t = h.reshape(-1, d)
    n_tok = t.shape[0]
    n_assign = n_tok * TOP_K
    logits = (t @ router_w + router_b).astype(jnp.float32)
    top_val, top_idx = lax.top_k(logits, TOP_K)
    top_w = jax.nn.softmax(top_val, axis=-1)
    flat_e = top_idx.reshape(-1)
    flat_t = jnp.repeat(jnp.arange(n_tok, dtype=jnp.int32), TOP_K)
    flat_w = top_w.reshape(-1)
    order = jnp.argsort(flat_e)
    e_sorted = flat_e[order]
    counts = jnp.bincount(flat_e, length=N_EXPERTS)
    starts = jnp.cumsum(counts) - counts
    padded = (counts + E_BLOCK - 1) // E_BLOCK * E_BLOCK
    pad_ends = jnp.cumsum(padded)
    pad_starts = pad_ends - padded
    dest = pad_starts[e_sorted] + (jnp.arange(n_assign) - starts[e_sorted])
    n_blocks = (n_assign + N_EXPERTS * (E_BLOCK - 1) + E_BLOCK - 1) // E_BLOCK
    cap = n_blocks * E_BLOCK
    slot_tok = jnp.full((cap,), n_tok, jnp.int32).at[dest].set(flat_t[order])
    slot_w = jnp.zeros((cap,), jnp.float32).at[dest].set(flat_w[order])
    block_e = jnp.minimum(jnp.searchsorted(pad_ends, jnp.arange(n_blocks) * E_BLOCK, side='right'),
                          N_EXPERTS - 1)
    t_pad = jnp.concatenate([t, jnp.zeros((1, d), t.dtype)], axis=0)
    xb = t_pad[slot_tok].reshape(n_blocks, E_BLOCK, d)

    def expert_block(args):
        xblk, e = args
        gu = xblk @ w_gu[e] + b_gu[e]
        glu, lin = jnp.split(gu, 2, axis=-1)
        glu = jnp.minimum(glu, SWIGLU_LIMIT)
        lin = jnp.clip(lin, -SWIGLU_LIMIT, SWIGLU_LIMIT)
        act = glu * jax.nn.sigmoid(SWIGLU_ALPHA * glu) * (lin + 1.0)
        return act @ w_down[e] + b_down[e]

    yb = lax.map(expert_block, (xb, block_e)).reshape(cap, d)
    y = jax.ops.segment_sum(yb * slot_w[:, None].astype(yb.dtype), slot_tok, num_segments=n_tok + 1)[:n_tok]
    return y.reshape(bsz, n, d)


def setup_inputs(seed: int = 0) -> dict:
    key = jax.random.key(seed)
    ks = iter(jax.random.split(key, 40))
    f32 = jnp.float32
    L, D = DEPTH, D_MODEL

    def normal(shape, scale):
        return jax.random.normal(next(ks), shape, f32) * scale

    def gain(n):
        return 1.0 + normal((L, n), 0.02)

    return {
        'x': normal((BATCH, SEQ, D), 1.0),
        'c': normal((BATCH, D), 1.0),
        'ctx': normal((BATCH, CTX_LEN, D), 1.0),
        'c_ctx': normal((D,), 1.0),
        'w_mod': normal((L, D, N_MOD * D), 0.5 * D ** -0.5),
        'b_mod': normal((L, N_MOD * D), 0.01),
        'norm_mix': gain(D),
        'norm_ffn': gain(D),
        'w_in': normal((L, D, IN_COLS), D ** -0.5),
        'a_q_norm': gain(A_DIM),
        'a_k_norm': gain(A_DIM),
        'b_q_a_norm': gain(B_Q_RANK),
        'b_kv_a_norm': gain(B_KV_RANK),
        'b_w_uq': normal((L, B_Q_RANK, B_HEADS * (B_NOPE + B_ROPE)), B_Q_RANK ** -0.5),
        'b_w_ukv': normal((L, B_KV_RANK, B_HEADS * (B_NOPE + B_V)), B_KV_RANK ** -0.5),
        'b_q_norm': gain(B_NOPE + B_ROPE),
        'b_k_norm': gain(B_NOPE + B_ROPE),
        'c_q_norm': gain(C_DIM),
        'c_k_norm': gain(C_DIM),
        'c_lambda': normal((L, 4, C_DIM), 0.1),
        'c_subln': gain(C_V),
        'w_o_a': normal((L, A_WIDTH, D), A_WIDTH ** -0.5),
        'w_o_b': normal((L, B_WIDTH, D), B_WIDTH ** -0.5),
        'w_o_c': normal((L, C_WIDTH, D), C_WIDTH ** -0.5),
        'w_out': normal((L, D, D), D ** -0.5),
        'router_w': normal((L, D, N_EXPERTS), D ** -0.5),
        'router_b': normal((L, N_EXPERTS), 0.01),
        'exp_w_gu': normal((L, N_EXPERTS, D, 2 * D_EXPERT), D ** -0.5),
        'exp_b_gu': normal((L, N_EXPERTS, 2 * D_EXPERT), 0.01),
        'exp_w_down': normal((L, N_EXPERTS, D_EXPERT, D), D_EXPERT ** -0.5),
        'exp_b_down': normal((L, N_EXPERTS, D), 0.01),
    }


def reference(x, c, ctx, c_ctx, w_mod, b_mod, norm_mix, norm_ffn, w_in, a_q_norm, a_k_norm,
              b_q_a_norm, b_kv_a_norm, b_w_uq, b_w_ukv, b_q_norm, b_k_norm, c_q_norm, c_k_norm,
              c_lambda, c_subln, w_o_a, w_o_b, w_o_c, w_out, router_w, router_b,
              exp_w_gu, exp_b_gu, exp_w_down, exp_b_down):
    n_lat = x.shape[1]
    n_ctx = ctx.shape[1]
    rope_a = axial_rope(n_lat, A_DIM)
    rope_b = axial_rope(n_lat, B_ROPE)
    rope_c = axial_rope(n_lat, C_DIM)
    x_lat, x_ctx = x, ctx
    for l in range(DEPTH):
        last = l == DEPTH - 1
        lam_init = 0.8 - 0.6 * math.exp(-0.3 * l)
        lq1, lk1, lq2, lk2 = c_lambda[l].astype(jnp.float32)
        lam = jnp.exp(jnp.sum(lq1 * lk1)) - jnp.exp(jnp.sum(lq2 * lk2)) + lam_init
        sh1, sc1, g1, sh2, sc2, g2 = adaln(c[:, None, :], w_mod[l], b_mod[l])
        csh1, csc1, cg1, csh2, csc2, cg2 = adaln(c_ctx, w_mod[l], b_mod[l])

        def feats(h, ra, rb, rc):
            return mixer_inputs(h, w_in[l], a_q_norm[l], a_k_norm[l], b_q_a_norm[l], b_kv_a_norm[l],
                                b_w_uq[l], b_w_ukv[l], b_q_norm[l], b_k_norm[l], c_q_norm[l],
                                c_k_norm[l], ra, rb, rc)

        def merge(outs, gates):
            return merge_branches(*outs, gates, c_subln[l], lam_init, w_o_a[l], w_o_b[l], w_o_c[l], w_out[l])

        h_lat = modulate(rms_norm(x_lat, norm_mix[l]), sh1, sc1)
        h_ctx = modulate(rms_norm(x_ctx, norm_mix[l]), csh1, csc1)
        q_lat, kv_lat, gate_lat = feats(h_lat, rope_a, rope_b, rope_c)
        q_ctx, kv_ctx, gate_ctx = feats(h_ctx, None, None, None)
        kv_all = tuple(jnp.concatenate([kc, kl], axis=1) for kc, kl in zip(kv_ctx, kv_lat))
        x_lat = x_lat + g1 * merge(token_mixers(q_lat, kv_all, lam, True), gate_lat)
        if not last:
            x_ctx = x_ctx + cg1 * merge(token_mixers(q_ctx, kv_ctx, lam, False), gate_ctx)

        moe = lambda h: moe_ffn(h, router_w[l], router_b[l], exp_w_gu[l], exp_b_gu[l],
                                exp_w_down[l], exp_b_down[l])
        h_lat = modulate(rms_norm(x_lat, norm_ffn[l]), sh2, sc2)
        if last:
            x_lat = x_lat + g2 * moe(h_lat)
        else:
            h_ctx = modulate(rms_norm(x_ctx, norm_ffn[l]), csh2, csc2)
            y = moe(jnp.concatenate([h_ctx, h_lat], axis=1))
            x_ctx = x_ctx + cg2 * y[:, :n_ctx]
            x_lat = x_lat + g2 * y[:, n_ctx:]
    return x_lat
```

```python
import numpy as np
import concourse.bass as bass
import concourse.mybir as mybir
from concourse.bass_utils import run_bass_kernel_spmd

F32 = mybir.dt.float32
BF16 = mybir.dt.bfloat16
ALU = mybir.AluOpType
AF = mybir.ActivationFunctionType
AX = mybir.AxisListType

L = 4
D = 1024
NLAT = 2048
NCTX = 256
NT = NCTX + NLAT
TBS = [(0, 256), (256, 512), (768, 512), (1280, 512), (1792, 512)]
GROUPS = [[0, 1, 2], [3, 4]]
NKT = 66
NKEY = NKT * 128
EPS = 1e-6
KT_ROWS = 1184
V_COLS = 1152
KV_ROWS = KT_ROWS + V_COLS
QT_ROWS = 1792
OT_ROWS = 1536
NG = 28
FMW = 2176
TMW = 640
WINP = FMW + TMW + 3072


class Sem:
    def __init__(self, h, step):
        self.h = h
        self.step = step
        self.n = 0
        self.signal = None


class Eng:
    def __init__(self, name, sem):
        self.name = name
        self.sem = sem
        self.prog = []
        self.seen = {}
        self.sig = set()


class Buf:
    def __init__(self, ap, dsem=None):
        self.ap = ap
        self.w = None
        self.r = {}
        self.dsem = dsem

    def __getitem__(self, k):
        return self.ap[k]


class Sched:
    def __init__(self, nc):
        self.nc = nc
        self.sems_used = 0
        self.dsems = []
        self.E = {}
        for name in ("pe", "act", "dve", "pool", "sp"):
            self.E[name] = Eng(name, self.new_sem(1))

    def new_sem(self, step):
        self.sems_used += 1
        sm = Sem(self.nc.alloc_semaphore(name=f"s{self.sems_used}"), step)
        if step == 16:
            self.dsems.append(sm)
        return sm

    def barrier(self):
        for E in self.E.values():
            waits = []
            for E2 in self.E.values():
                if E2 is E or E2.sem.n == 0:
                    continue
                if E.seen.get(E2.sem, 0) < E2.sem.n:
                    E.seen[E2.sem] = E2.sem.n
                    waits.append((E2.sem, E2.sem.n))
                    E2.sig.add(E2.sem.n)
            for sm in self.dsems:
                if sm.n > 0 and E.seen.get(sm, 0) < sm.n:
                    E.seen[sm] = sm.n
                    waits.append((sm, sm.n))
            E.prog.append((waits, None, None, None))

    def _waits(self, E, reads, writes):
        deps = {}

        def add(t):
            if t is None:
                return
            s, n = t
            if s is E.sem and E.name in ("pe", "sp"):
                return
            if deps.get(s, 0) < n:
                deps[s] = n

        for b in reads:
            add(b.w)
        for b in writes:
            add(b.w)
            for s, n in b.r.items():
                add((s, n))
        waits = []
        for s, n in deps.items():
            if E.seen.get(s, 0) < n:
                E.seen[s] = n
                waits.append((s, n))
                if s.step == 1:
                    s.owner.sig.add(n)
        return waits

    def op(self, ename, fn, reads=(), writes=()):
        E = self.E[ename]
        waits = self._waits(E, reads, writes)
        E.sem.n += 1
        n = E.sem.n
        E.prog.append((waits, fn, n, None))
        for b in reads:
            b.r[E.sem] = n
        for b in writes:
            b.w = (E.sem, n)
            b.r = {}

    def dma(self, ename, out_ap, in_ap, sem, reads=(), writes=()):
        E = self.E[ename]
        waits = self._waits(E, reads, writes)
        sem.n += 1
        n = sem.n
        E.prog.append((waits, lambda e: e.dma_start(out=out_ap, in_=in_ap), None, (sem, n)))
        for b in reads:
            b.r[sem] = n
        for b in writes:
            b.w = (sem, n)
            b.r = {}

    def finish(self, ename, bufs):
        E = self.E[ename]
        waits = self._waits(E, bufs, [])
        E.prog.append((waits, None, None, None))

    def emit(self):
        for E in self.E.values():
            E.sem.owner = E
        for E in self.E.values():
            E.sigl = sorted(E.sig)
            E.rank = {n: i + 1 for i, n in enumerate(E.sigl)}

        def val(s, n):
            if s.step == 16:
                return 16 * n
            return s.owner.rank[n]

        def run(E, e):
            for waits, fn, n, dm in E.prog:
                for s, nn in waits:
                    e.wait_ge(s.h, val(s, nn))
                if fn is None:
                    continue
                ins = fn(e)
                if dm is not None:
                    ins.then_inc(dm[0].h, 16)
                elif n in E.sig:
                    ins.then_inc(E.sem.h, 1)

        with self.nc.Block() as block:
            @block.tensor
            def _(e):
                run(self.E["pe"], e)

            @block.scalar
            def _(e):
                run(self.E["act"], e)

            @block.vector
            def _(e):
                run(self.E["dve"], e)

            @block.gpsimd
            def _(e):
                run(self.E["pool"], e)

            @block.sync
            def _(e):
                run(self.E["sp"], e)


class Pool:
    def __init__(self, bufs):
        self.bufs = bufs
        self.i = 0

    def next(self):
        b = self.bufs[self.i % len(self.bufs)]
        self.i += 1
        return b


def build_program(mode, layers, n_ring=4, dbg=None):
    nc = bass.Bass("TRN2", target_bir_lowering=False)
    S = Sched(nc)
    for E in S.E.values():
        E.sem.owner = E
    nl = len(layers)

    def din(name, shape, dt=F32):
        return nc.dram_tensor(name, list(shape), dt, kind="ExternalInput").ap()

    def dout(name, shape, dt=F32):
        return nc.dram_tensor(name, list(shape), dt, kind="ExternalOutput").ap()

    def dint(name, shape, dt=BF16):
        if dbg:
            return nc.dram_tensor(name, list(shape), dt, kind="ExternalOutput").ap()
        return nc.dram_tensor(name, list(shape), dt, kind="Internal").ap()

    xT_in = din("xT_in", [128, 8, NT])
    condT = din("condT", [128, 8, 2])
    cosA_d = din("cosA", [128, NLAT]); sinA_d = din("sinA", [128, NLAT])
    cosB_d = din("cosB", [128, NLAT]); sinB_d = din("sinB", [128, NLAT])
    cmat_d = din("cmat", [128, 6, 128])
    gmul_d = din("gmul", [nl, 128, NG])
    gains_d = din("gains", [nl, 128, NG])
    clam_d = din("clam", [nl, 128, 128])
    laminit_d = din("laminit", [nl, 128, 1])
    bmod_d = din("bmod", [nl, 128, 48])
    wmod_d = din("wmod", [nl, 1024, 6144])
    winp_d = din("winp", [nl, 1024, WINP])
    wuq_d = din("wuq", [nl, 256, 768])
    wukv_d = din("wukv", [nl, 128, 1024])
    if mode != "A":
        rb_d = din("rb", [nl, 128, 32])
        bgu_d = din("bgu", [nl, 128, 32, 16])
        bdn_d = din("bdn", [nl, 32, 1024])
        rw_d = din("rw", [nl, 128, 8, 32])
        woa_d = din("woa", [nl, 512, 1024]); wob_d = din("wob", [nl, 512, 1024]); woc_d = din("woc", [nl, 512, 1024])
        wout_d = din("wout", [nl, 1024, 1024])
        wgu_d = din("wgu", [nl, 32, 1024, 2048])
        wdn_d = din("wdn", [nl, 32, 1024, 1024])

    if mode == "A":
        kv_loc = dout("kv_loc", [KV_ROWS, NLAT], BF16)
    else:
        kv_loc = dint("kv_loc", [KV_ROWS, NLAT])
    if mode == "B":
        kv_all = din("kv_all", [4 * KV_ROWS, NLAT], BF16)
    elif mode == "fused":
        kv_all = nc.dram_tensor("kv_all", [4 * KV_ROWS, NLAT], BF16, kind="Internal").ap()
    else:
        kv_all = None
    ktc_d = dint("ktc", [KT_ROWS, NCTX])
    vc_d = dint("vc", [NCTX, V_COLS])
    qt_d = dint("qt", [QT_ROWS, NT])
    ot_d = dint("ot", [OT_ROWS, NT])
    if mode != "A":
        out_d = dout("out", [128, 8, NT])

    kvloc_b = Buf(kv_loc); kvall_b = Buf(kv_all); ktc_b = Buf(ktc_d); vc_b = Buf(vc_d)
    qt_b = Buf(qt_d); ot_b = Buf(ot_d)

    cnt = [0]

    def sb(shape, dt, dma=False):
        cnt[0] += 1
        h = nc.alloc_sbuf_tensor(f"t{cnt[0]}", list(shape), dt)
        return Buf(h, S.new_sem(16) if dma else None)

    def sbpool(n, shape, dt, dma=False):
        return Pool([sb(shape, dt, dma) for _ in range(n)])

    USIZE = 36352
    phase_sems = []
    ps_idx = [0]

    def psem():
        if ps_idx[0] == len(phase_sems):
            phase_sems.append(S.new_sem(16))
        sm = phase_sems[ps_idx[0]]
        ps_idx[0] += 1
        return sm

    def phase_start():
        S.barrier()
        uoff[0] = 0
        ps_idx[0] = 0

    U = nc.alloc_sbuf_tensor("union", [128, USIZE], BF16)
    UF = U[:, :].bitcast(F32)
    uoff = [0]

    def carve(shape, dt, dma=False):
        per = 1
        for d_ in shape[1:]:
            per *= d_
        cols = per * (2 if dt == F32 else 1)
        cols_al = (cols + 15) // 16 * 16
        o = uoff[0]
        assert o + cols_al <= USIZE, ("union overflow", o, cols_al)
        uoff[0] = o + cols_al
        if dt == F32:
            ap = UF[0:shape[0], o // 2:(o + cols) // 2]
        else:
            ap = U[0:shape[0], o:o + cols]
        if len(shape) == 3:
            ap = ap.rearrange("p (a b) -> p a b", b=shape[2])
        elif len(shape) == 4:
            ap = ap.rearrange("p (a b c) -> p a b c", b=shape[2], c=shape[3])
        return Buf(ap, psem() if dma else None)

    banks = []
    for i in range(8):
        banks.append(Buf(nc.alloc_psum_tensor(f"ps{i}", [128, 512], F32)))
    PA = Pool(banks[0:3]); PB = Pool(banks[3:5]); PC = Pool(banks[5:7]); PD = Pool(banks[7:8])

    xT = [sb([128, 8, w], F32, dma=True) for (_, w) in TBS]
    cmat = sb([128, 6, 128], BF16, dma=True)
    c32 = sb([128, 2, 128], F32, dma=True)
    ONES, BLK64, BLK32, R64, R32, IDENT = range(6)
    silu_c = sb([128, 8, 2], BF16)
    cond32 = sb([128, 8, 2], F32, dma=True)
    modT = [sb([128, 48, 2], F32) for _ in layers]
    gains = [sb([128, NG], F32, dma=True) for _ in layers]
    gmul = sb([128, NG], F32, dma=True)
    bmod = sb([128, 48], F32, dma=True)
    AB = [sb([128, 6, 8, 2], F32) for _ in layers]
    neglam = [sb([128, 1], F32) for _ in layers]
    clam = sb([128, 128], F32, dma=True)
    laminit = sb([128, 1], F32, dma=True)
    lamtmp = sb([128, 68], F32)

    RING = nc.alloc_sbuf_tensor("ring", [128, n_ring * 4096], BF16)
    ring = Pool([Buf(RING[:, i * 4096:(i + 1) * 4096].rearrange("p (c n) -> p c n", n=512), S.new_sem(16)) for i in range(n_ring)])
    sq_p = sbpool(2, [128, 512], BF16)
    f32_p = sbpool(5, [128, 512], F32)
    sig_p = sbpool(2, [128, 512], F32)

    def dma_in(buf, dst_ap, src_ap, q="sp", extra_reads=()):
        S.dma(q, dst_ap, src_ap, buf.dsem, reads=list(extra_reads), writes=[buf])

    def dma_out(dst_buf, dst_ap, src_buf, src_ap, q="sp"):
        S.dma(q, dst_ap, src_ap, src_buf.dsem, reads=[src_buf], writes=[dst_buf])

    def mm(ps, ps_ap, lhsT_buf, lhsT_ap, rhs_buf, rhs_ap, start, stop):
        S.op("pe", lambda e: e.matmul(ps_ap, lhsT_ap, rhs_ap, start=start, stop=stop),
             reads=[lhsT_buf, rhs_buf], writes=[ps])

    for bi, (t0, w) in enumerate(TBS):
        dma_in(xT[bi], xT[bi][:, :, :], xT_in[:, :, t0:t0 + w])
    dma_in(cmat, cmat[:, :, :], cmat_d[:, :, :], q="pool")
    dma_in(c32, c32[:, 0, :], cmat_d[:, 0, :])
    dma_in(c32, c32[:, 1, :], cmat_d[:, 5, :])
    dma_in(cond32, cond32[:, :, :], condT[:, :, :])
    S.op("act", lambda e: e.activation(silu_c[:, :, :], cond32[:, :, :], AF.Silu), reads=[cond32], writes=[silu_c])

    def wchunk(w2d, c0, ncols, krows=1024):
        kc = krows // 128
        src = w2d[:, c0:c0 + ncols].rearrange("(c p) n -> p c n", p=128)
        b = ring.next()
        S.dma("pool", b[:, 0:kc, 0:ncols], src, b.dsem, reads=[], writes=[b])
        return b

    def emit_modulation(li):
        l_w = wmod_d[li]
        dma_in(gains[li], gains[li][:, :], gains_d[li])
        dma_in(gmul, gmul[:, :], gmul_d[li])
        dma_in(bmod, bmod[:, :], bmod_d[li])
        ps = PD.next()
        for cc in range(12):
            wb = wchunk(l_w, cc * 512, 512)
            for j4 in range(4):
                j = cc * 4 + j4
                for dc in range(8):
                    mm(ps, ps[:, 2 * j:2 * j + 2], wb, wb[:, dc, j4 * 128:(j4 + 1) * 128],
                       silu_c, silu_c[:, dc, :], dc == 0, dc == 7)
        m = modT[li]
        S.op("dve", lambda e: e.tensor_tensor(
            m[:, :, :], ps[:, 0:96].rearrange("p (j k) -> p j k", k=2),
            bmod[:, :].unsqueeze(2).to_broadcast([128, 48, 2]), ALU.add),
            reads=[ps, bmod], writes=[m])
        g = gains[li]
        S.op("dve", lambda e: e.tensor_tensor(g[:, :], g[:, :], gmul[:, :], ALU.mult),
             reads=[g, gmul], writes=[g])
        ab = AB[li]
        for k, (sc_j, sh_j, g_j, gcol) in enumerate([(8, 0, 16, 0), (32, 24, 40, 8)]):
            S.op("dve", lambda e, k=k, sc_j=sc_j, gcol=gcol: e.scalar_tensor_tensor(
                ab[:, 3 * k + 0, :, :], m[:, sc_j:sc_j + 8, :], 1.0,
                g[:, gcol:gcol + 8].unsqueeze(2).to_broadcast([128, 8, 2]), ALU.add, ALU.mult),
                reads=[m, g], writes=[ab])
            S.op("dve", lambda e, k=k, sh_j=sh_j: e.tensor_copy(ab[:, 3 * k + 1, :, :], m[:, sh_j:sh_j + 8, :]),
                 reads=[m], writes=[ab])
            S.op("dve", lambda e, k=k, g_j=g_j: e.tensor_copy(ab[:, 3 * k + 2, :, :], m[:, g_j:g_j + 8, :]),
                 reads=[m], writes=[ab])
        dma_in(clam, clam[:, :], clam_d[li])
        dma_in(laminit, laminit[:, :], laminit_d[li])
        lt = lamtmp
        S.op("dve", lambda e: e.tensor_tensor(lt[:, 0:32], clam[:, 0:32], clam[:, 32:64], ALU.mult), reads=[clam], writes=[lt])
        S.op("dve", lambda e: e.tensor_tensor(lt[:, 32:64], clam[:, 64:96], clam[:, 96:128], ALU.mult), reads=[clam], writes=[lt])
        S.op("dve", lambda e: e.reduce_sum(lt[:, 64:65], lt[:, 0:32], AX.X), reads=[lt], writes=[lt])
        S.op("dve", lambda e: e.reduce_sum(lt[:, 65:66], lt[:, 32:64], AX.X), reads=[lt], writes=[lt])
        S.op("act", lambda e: e.activation(lt[:, 66:68], lt[:, 64:66], AF.Exp), reads=[lt], writes=[lt])
        nlm = neglam[li]
        S.op("dve", lambda e: e.tensor_tensor(nlm[:, :], lt[:, 67:68], lt[:, 66:67], ALU.subtract), reads=[lt], writes=[nlm])
        S.op("dve", lambda e: e.tensor_tensor(nlm[:, :], nlm[:, :], laminit[:, :], ALU.subtract), reads=[nlm, laminit], writes=[nlm])

    def emit_norm_mod(li, bi, which, dst, dst_off, f32_dst=None, f32_deps=()):
        t0, w = TBS[bi]
        cond = 1 if bi == 0 else 0
        ab = AB[li]
        x = xT[bi]
        ps = PD.next()
        for dc in range(8):
            sq = sq_p.next()
            S.op("act", lambda e, sq=sq, dc=dc: e.activation(sq[:, 0:w], x[:, dc, :], AF.Square), reads=[x], writes=[sq])
            mm(ps, ps[:, 0:w], cmat, cmat[:, ONES, :], sq, sq[:, 0:w], dc == 0, dc == 7)
        rstd = f32_p.next()
        S.op("act", lambda e: e.activation(rstd[:, 0:w], ps[:, 0:w], AF.Sqrt, bias=EPS * D, scale=1.0), reads=[ps], writes=[rstd])
        S.op("dve", lambda e: e.reciprocal(rstd[:, 0:w], rstd[:, 0:w]), reads=[rstd], writes=[rstd])
        fd = list(f32_deps)
        for dc in range(8):
            tmp = sig_p.next()
            S.op("dve", lambda e, tmp=tmp, dc=dc: e.scalar_tensor_tensor(
                tmp[:, 0:w], x[:, dc, :], ab[:, 3 * which, dc, cond:cond + 1], rstd[:, 0:w], ALU.mult, ALU.mult),
                reads=[x, ab, rstd], writes=[tmp])
            if f32_dst is not None:
                S.op("act", lambda e, tmp=tmp, dc=dc: e.activation(
                    f32_dst[:, dc, 0:w], tmp[:, 0:w], AF.Identity, bias=ab[:, 3 * which + 1, dc, cond:cond + 1]),
                    reads=[tmp, ab], writes=[f32_dst] + fd)
                S.op("pool", lambda e, dc=dc: e.tensor_copy(dst[:, dc, dst_off:dst_off + w], f32_dst[:, dc, 0:w]),
                     reads=[f32_dst] + fd, writes=[dst])
            else:
                S.op("act", lambda e, tmp=tmp, dc=dc: e.activation(
                    dst[:, dc, dst_off:dst_off + w], tmp[:, 0:w], AF.Identity, bias=ab[:, 3 * which + 1, dc, cond:cond + 1]),
                    reads=[tmp, ab], writes=[dst])

    def group_range(g):
        t0 = TBS[GROUPS[g][0]][0]
        t1 = TBS[GROUPS[g][-1]][0] + TBS[GROUPS[g][-1]][1]
        return t0, t1

    def emit_phase_P(li, g):
        phase_start()
        bf_p = Pool([carve([128, 512], BF16, dma=True) for _ in range(3)])
        st_p = Pool([carve([128, 512], BF16, dma=True) for _ in range(3)])
        hT = carve([128, 8, 1280], BF16)
        cosA = carve([128, 1024], F32, dma=True); sinA = carve([128, 1024], F32, dma=True)
        cosB = carve([128, 1024], F32, dma=True); sinB = carve([128, 1024], F32, dma=True)
        cqn = carve([128, 2, 1280], BF16)
        ckvn = carve([128, 1280], BF16)
        wuq = carve([128, 2, 768], BF16, dma=True)
        wukv = carve([128, 1024], BF16, dma=True)
        gg = gains[li]

        gt0, gt1 = group_range(g)
        blocks = GROUPS[g]
        lat0 = max(gt0, NCTX) - NCTX
        latn = gt1 - NCTX - lat0
        for tb_, src in ((cosA, cosA_d), (sinA, sinA_d), (cosB, cosB_d), (sinB, sinB_d)):
            dma_in(tb_, tb_[:, 0:latn], src[:, lat0:lat0 + latn])
        dma_in(wuq, wuq[:, :, :], wuq_d[li].rearrange("(c p) n -> p c n", p=128), q="pool")
        dma_in(wukv, wukv[:, :], wukv_d[li], q="pool")
        for bi in blocks:
            emit_norm_mod(li, bi, 0, hT, TBS[bi][0] - gt0)
        w_l = winp_d[li]
        ropeA = (R64, cosA, sinA)
        ropeB = (R32, cosB, sinB)

        def fm_finish(raw_ps, rows, w, blk, dim, gcol, rope, lat_off, dsts, is_ctx):
            sq = sq_p.next()
            S.op("act", lambda e: e.activation(sq[0:rows, 0:w], raw_ps[0:rows, 0:w], AF.Square), reads=[raw_ps], writes=[sq])
            ss = PB.next()
            mm(ss, ss[0:rows, 0:w], cmat, cmat[0:rows, blk, 0:rows], sq, sq[0:rows, 0:w], True, True)
            rstd = f32_p.next()
            S.op("act", lambda e: e.activation(rstd[0:rows, 0:w], ss[0:rows, 0:w], AF.Sqrt, bias=EPS * dim, scale=1.0), reads=[ss], writes=[rstd])
            S.op("dve", lambda e: e.reciprocal(rstd[0:rows, 0:w], rstd[0:rows, 0:w]), reads=[rstd], writes=[rstd])
            qn = bf_p.next()
            S.op("dve", lambda e: e.scalar_tensor_tensor(qn[0:rows, 0:w], raw_ps[0:rows, 0:w], gg[0:rows, gcol:gcol + 1],
                                                        rstd[0:rows, 0:w], ALU.mult, ALU.mult),
                 reads=[raw_ps, gg, rstd], writes=[qn])
            if rope is not None and not is_ctx:
                rmat, cs, sn = rope
                rot = PC.next()
                mm(rot, rot[0:rows, 0:w], cmat, cmat[0:rows, rmat, 0:rows], qn, qn[0:rows, 0:w], True, True)
                t1 = f32_p.next()
                S.op("pool", lambda e: e.tensor_tensor(t1[0:rows, 0:w], qn[0:rows, 0:w], cs[0:rows, lat_off:lat_off + w], ALU.mult),
                     reads=[qn, cs], writes=[t1])
                t2 = f32_p.next()
                S.op("dve", lambda e: e.tensor_tensor(t2[0:rows, 0:w], rot[0:rows, 0:w], sn[0:rows, lat_off:lat_off + w], ALU.mult),
                     reads=[rot, sn], writes=[t2])
                ob = st_p.next()
                S.op("pool", lambda e: e.tensor_tensor(ob[0:rows, 0:w], t1[0:rows, 0:w], t2[0:rows, 0:w], ALU.add),
                     reads=[t1, t2], writes=[ob])
            else:
                ob = qn
            for (r0, r1, dbuf, dap) in dsts:
                dma_out(dbuf, dap, ob, ob[r0:r1, 0:w])

        def kdst(bi, r0, nrows, krow0):
            t0, w = TBS[bi]
            if bi == 0:
                return (r0, r0 + nrows, ktc_b, ktc_d[krow0:krow0 + nrows, 0:w])
            return (r0, r0 + nrows, kvloc_b, kv_loc[krow0:krow0 + nrows, t0 - NCTX:t0 - NCTX + w])

        def qdst(bi, r0, nrows, qrow0):
            t0, w = TBS[bi]
            return (r0, r0 + nrows, qt_b, qt_d[qrow0:qrow0 + nrows, t0:t0 + w])

        for rc0 in range(0, 17, 4):
            nrc = min(4, 17 - rc0)
            wb = wchunk(w_l, rc0 * 128, nrc * 128)
            for bi in blocks:
                t0, w = TBS[bi]
                ho = t0 - gt0
                lo = t0 - NCTX - lat0
                ctxb = (bi == 0)
                raw5 = None
                for r in range(nrc):
                    idx = rc0 + r
                    raw = PA.next()
                    for dc in range(8):
                        mm(raw, raw[:, 0:w], wb, wb[:, dc, r * 128:(r + 1) * 128], hT, hT[:, dc, ho:ho + w], dc == 0, dc == 7)
                    if idx < 4:
                        fm_finish(raw, 128, w, BLK64, 64, 16, ropeA, lo, [qdst(bi, 0, 128, idx * 128)], ctxb)
                    elif idx == 4:
                        fm_finish(raw, 128, w, BLK64, 64, 17, ropeA, lo, [kdst(bi, 0, 128, 0)], ctxb)
                    elif idx == 5:
                        raw5 = raw
                    elif idx == 6:
                        ss = PB.next()
                        for k, rp in enumerate((raw5, raw)):
                            sq = sq_p.next()
                            S.op("act", lambda e, sq=sq, rp=rp, w=w: e.activation(sq[:, 0:w], rp[:, 0:w], AF.Square), reads=[rp], writes=[sq])
                            mm(ss, ss[:, 0:w], cmat, cmat[:, ONES, :], sq, sq[:, 0:w], k == 0, k == 1)
                        rstd = f32_p.next()
                        S.op("act", lambda e, rstd=rstd, ss=ss, w=w: e.activation(rstd[:, 0:w], ss[:, 0:w], AF.Sqrt, bias=EPS * 256, scale=1.0), reads=[ss], writes=[rstd])
                        S.op("dve", lambda e, rstd=rstd, ss=ss, w=w: e.reciprocal(rstd[:, 0:w], rstd[:, 0:w]), reads=[rstd], writes=[rstd])
                        for k, rp in enumerate((raw5, raw)):
                            S.op("dve", lambda e, k=k, rp=rp, rstd=rstd, w=w, ho=ho: e.scalar_tensor_tensor(
                                cqn[:, k, ho:ho + w], rp[:, 0:w], gg[:, 18 + k:19 + k], rstd[:, 0:w], ALU.mult, ALU.mult),
                                reads=[rp, gg, rstd], writes=[cqn])
                    elif idx == 7:
                        sq = sq_p.next()
                        S.op("act", lambda e, sq=sq, raw=raw, w=w: e.activation(sq[:, 0:w], raw[:, 0:w], AF.Square), reads=[raw], writes=[sq])
                        ss = PB.next()
                        mm(ss, ss[:, 0:w], cmat, cmat[:, ONES, :], sq, sq[:, 0:w], True, True)
                        rstd = f32_p.next()
                        S.op("act", lambda e, rstd=rstd, ss=ss, w=w: e.activation(rstd[:, 0:w], ss[:, 0:w], AF.Sqrt, bias=EPS * 128, scale=1.0), reads=[ss], writes=[rstd])
                        S.op("dve", lambda e, rstd=rstd, ss=ss, w=w: e.reciprocal(rstd[:, 0:w], rstd[:, 0:w]), reads=[rstd], writes=[rstd])
                        S.op("dve", lambda e, raw=raw, rstd=rstd, w=w, ho=ho: e.scalar_tensor_tensor(
                            ckvn[:, ho:ho + w], raw[:, 0:w], gg[:, 20:21], rstd[:, 0:w], ALU.mult, ALU.mult),
                            reads=[raw, gg, rstd], writes=[ckvn])
                    elif idx < 12:
                        fm_finish(raw, 128, w, BLK32, 32, 25, ropeB, lo, [qdst(bi, 0, 128, 1280 + (idx - 8) * 128)], ctxb)
                    elif idx < 16:
                        fm_finish(raw, 128, w, BLK32, 32, 26, ropeB, lo, [kdst(bi, 0, 128, 672 + (idx - 12) * 128)], ctxb)
                    else:
                        fm_finish(raw, 32, w, BLK32, 32, 24, ropeB, lo, [kdst(bi, 0, 32, 640)], ctxb)
        wv1 = wchunk(w_l, FMW, 512)
        wv2 = wchunk(w_l, FMW + 512, 128)
        vreg = kv_loc[KT_ROWS:KV_ROWS, :].rearrange("r c -> (r c)").rearrange("(t v) -> t v", v=V_COLS)

        def vdst(bi, s_, c0, ncol):
            t0, w = TBS[bi]
            if bi == 0:
                return (vc_b, vc_d[s_ * 128:(s_ + 1) * 128, c0:c0 + ncol])
            tl = t0 - NCTX + s_ * 128
            return (kvloc_b, vreg[tl:tl + 128, c0:c0 + ncol])

        def tm_chunk(bi, s_, lhs_buf, lhs_aps, rhs_buf, rhs_aps, ncol, vcol0):
            ps = PA.next()
            nk = len(lhs_aps)
            for k in range(nk):
                mm(ps, ps[:, 0:ncol], lhs_buf, lhs_aps[k], rhs_buf, rhs_aps[k], k == 0, k == nk - 1)
            ob = st_p.next()
            S.op("act", lambda e: e.copy(ob[:, 0:ncol], ps[:, 0:ncol]), reads=[ps], writes=[ob])
            dbuf, dap = vdst(bi, s_, vcol0, ncol)
            dma_out(dbuf, dap, ob, ob[:, 0:ncol])

        for bi in blocks:
            t0, w = TBS[bi]
            ho = t0 - gt0
            for s_ in range(w // 128):
                hs = [hT[:, k, ho + s_ * 128:ho + (s_ + 1) * 128] for k in range(8)]
                tm_chunk(bi, s_, hT, hs, wv1, [wv1[:, k, 0:128] for k in range(8)], 128, 0)
                tm_chunk(bi, s_, hT, hs, wv1, [wv1[:, k, 128:512] for k in range(8)], 384, 640)
                tm_chunk(bi, s_, hT, hs, wv2, [wv2[:, k, 0:128] for k in range(8)], 128, 640 + 384)
        for bi in blocks:
            t0, w = TBS[bi]
            ho = t0 - gt0
            lo = t0 - NCTX - lat0
            ctxb = (bi == 0)
            for i in range(4):
                raw = PA.next()
                for k in range(2):
                    mm(raw, raw[:, 0:w], wuq, wuq[:, k, i * 128:(i + 1) * 128], cqn, cqn[:, k, ho:ho + w], k == 0, k == 1)
                fm_finish(raw, 128, w, BLK64, 64, 21, None, lo,
                          [qdst(bi, 0, 64, 512 + (2 * i) * 96), qdst(bi, 64, 64, 512 + (2 * i + 1) * 96)], ctxb)
            for i in range(2):
                raw = PA.next()
                for k in range(2):
                    mm(raw, raw[:, 0:w], wuq, wuq[:, k, 512 + i * 128:512 + (i + 1) * 128], cqn, cqn[:, k, ho:ho + w], k == 0, k == 1)
                fm_finish(raw, 128, w, BLK32, 32, 22, ropeB, lo,
                          [qdst(bi, 32 * j, 32, 512 + (4 * i + j) * 96 + 64) for j in range(4)], ctxb)
            for i in range(4):
                raw = PA.next()
                mm(raw, raw[:, 0:w], wukv, wukv[:, i * 128:(i + 1) * 128], ckvn, ckvn[:, ho:ho + w], True, True)
                fm_finish(raw, 128, w, BLK64, 64, 23, None, lo, [kdst(bi, 0, 128, 128 + i * 128)], ctxb)
            for s_ in range(w // 128):
                tm_chunk(bi, s_, ckvn, [ckvn[:, ho + s_ * 128:ho + (s_ + 1) * 128]], wukv, [wukv[:, 512:1024]], 512, 128)
        if dbg and dbg.get("dumpP") and g == 0:
            S.barrier()
            dsm = S.new_sem(16)
            for nm, bf_, shp in (("d_cqn", cqn, [128, 2, 1280]), ("d_ckvn", ckvn, [128, 1280]), ("d_wuq", wuq, [128, 2, 768]),
                                 ("d_wukv", wukv, [128, 1024]), ("d_hT", hT, [128, 8, 1280])):
                dd = nc.dram_tensor(nm, shp, BF16, kind="ExternalOutput").ap()
                idx_ = tuple(slice(None) for _ in shp)
                S.dma("sp", dd[idx_], bf_[idx_], dsm, reads=[bf_], writes=[])
            print("sbuf remaining", nc.sbuf_bytes_remaining)

    def head_table():
        hs = []
        for h in range(8):
            hs.append(dict(kind="A", krows=[((h // 4) * 64, 64)], vcol=(h // 4) * 64, qrows=[(h * 64, 64)],
                           scale=64 ** -0.5, orow=h * 64, maps=[(0, 64)]))
        for h in range(8):
            hs.append(dict(kind="B", krows=[(128 + h * 64, 64), (640, 32)], vcol=128 + h * 64,
                           qrows=[(512 + h * 96, 96)], scale=96 ** -0.5, orow=512 + h * 64, maps=[(0, 96)]))
        for h in range(8):
            hs.append(dict(kind="C", krows=[(672 + 2 * h * 32, 64)], vcol=640 + h * 64,
                           qrows=[(1280 + 2 * h * 32, 64)], scale=32 ** -0.5, orow=1024 + h * 64, maps=[(0, 32), (32, 32)]))
        return hs

    def emit_attention(li, last, heads=None):
        phase_start()
        ktb = [Buf(RING[0:96, j * 8192:(j + 1) * 8192], psem()) for j in range(2)]
        kcb = [carve([96, NCTX], BF16, dma=True) for _ in range(2)]
        vtb = [carve([128, NKT, 65], BF16, dma=True) for _ in range(2)]
        qtb = [carve([96, NT], BF16, dma=True) for _ in range(2)]
        pt_p = Pool([carve([128, 512], BF16) for _ in range(4)])
        osb_p = Pool([carve([128, 512], F32) for _ in range(2)])
        rl_p = Pool([carve([128, 512], F32) for _ in range(2)])
        o_p = Pool([carve([64, 512], F32) for _ in range(4)])
        on_p = Pool([carve([64, 512], BF16, dma=True) for _ in range(3)])
        gg = gains[li]
        nlm = neglam[li]
        hs = head_table()
        if heads is not None:
            hs = [hs[i] for i in heads]
        for vb in vtb:
            S.op("pool", lambda e, vb=vb: e.memset(vb[:, :, 64:65], 1.0), reads=[], writes=[vb])
        vreg_all = [kv_all[r * KV_ROWS + KT_ROWS:(r + 1) * KV_ROWS, :].rearrange("r c -> (r c)").rearrange("(t v) -> t v", v=V_COLS)
                    for r in range(4)]
        for hi, hd in enumerate(hs):
            kb = ktb[hi % 2]; kc = kcb[hi % 2]; vb = vtb[hi % 2]; qb = qtb[hi % 2]
            r_off = 0
            for (kr0, nr) in hd["krows"]:
                S.dma("sp", kc[r_off:r_off + nr, :], ktc_d[kr0:kr0 + nr, :], kc.dsem, reads=[ktc_b], writes=[kc])
                for r in range(4):
                    S.dma("sp", kb[r_off:r_off + nr, r * NLAT:(r + 1) * NLAT],
                          kv_all[r * KV_ROWS + kr0:r * KV_ROWS + kr0 + nr, :], kb.dsem, reads=[kvall_b], writes=[kb])
                r_off += nr
            vc0 = hd["vcol"]
            S.dma("sp", vb[:, 0:2, 0:64], vc_d[:, vc0:vc0 + 64].rearrange("(k p) c -> p k c", p=128), vb.dsem,
                  reads=[vc_b], writes=[vb])
            for r in range(4):
                S.dma("sp", vb[:, 2 + 16 * r:2 + 16 * (r + 1), 0:64],
                      vreg_all[r][:, vc0:vc0 + 64].rearrange("(k p) c -> p k c", p=128), vb.dsem,
                      reads=[kvall_b], writes=[vb])
            r_off = 0
            for (qr0, nr) in hd["qrows"]:
                S.dma("sp", qb[r_off:r_off + nr, :], qt_d[qr0:qr0 + nr, :], qb.dsem, reads=[qt_b], writes=[qb])
                r_off += nr
            orow = hd["orow"]
            for bi, (t0, w) in enumerate(TBS):
                if bi == 0 and last:
                    continue
                nkt = 2 if bi == 0 else NKT
                onorm = []
                for (mr0, dk) in hd["maps"]:
                    ops = PB.next()
                    for kt in range(nkt):
                        sps = PA.next()
                        if kt < 2:
                            mm(sps, sps[:, 0:w], kc, kc[mr0:mr0 + dk, kt * 128:(kt + 1) * 128], qb, qb[mr0:mr0 + dk, t0:t0 + w], True, True)
                        else:
                            mm(sps, sps[:, 0:w], kb, kb[mr0:mr0 + dk, (kt - 2) * 128:(kt - 1) * 128], qb, qb[mr0:mr0 + dk, t0:t0 + w], True, True)
                        pt = pt_p.next()
                        S.op("act", lambda e, pt=pt, sps=sps, w=w, sc=hd["scale"]: e.activation(pt[:, 0:w], sps[:, 0:w], AF.Exp, scale=sc),
                             reads=[sps], writes=[pt])
                        mm(ops, ops[0:65, 0:w], vb, vb[:, kt, 0:65], pt, pt[:, 0:w], kt == 0, kt == nkt - 1)
                    rl = rl_p.next()
                    S.op("dve", lambda e, rl=rl, ops=ops, w=w: e.reciprocal(rl[64:65, 0:w], ops[64:65, 0:w]), reads=[ops], writes=[rl])
                    osb = osb_p.next()
                    S.op("act", lambda e, osb=osb, ops=ops, w=w: e.copy(osb[0:64, 0:w], ops[0:64, 0:w]), reads=[ops], writes=[osb])
                    bc = PC.next()
                    mm(bc, bc[0:64, 0:w], c32, c32[64:65, 0, 0:64], rl, rl[64:65, 0:w], True, True)
                    if hd["kind"] == "C":
                        o_ = o_p.next()
                    else:
                        o_ = on_p.next()
                    S.op("dve", lambda e, o_=o_, osb=osb, bc=bc, w=w: e.tensor_tensor(o_[:, 0:w], osb[0:64, 0:w], bc[0:64, 0:w], ALU.mult),
                         reads=[osb, bc], writes=[o_])
                    onorm.append(o_)
                if hd["kind"] == "C":
                    o1b, o2 = onorm
                    od = o_p.next()
                    S.op("dve", lambda e, od=od, o2=o2, o1b=o1b, w=w: e.scalar_tensor_tensor(
                        od[:, 0:w], o2[:, 0:w], nlm[0:64, 0:1], o1b[:, 0:w], ALU.mult, ALU.add),
                        reads=[o2, o1b, nlm], writes=[od])
                    sq = sq_p.next()
                    S.op("act", lambda e, sq=sq, od=od, w=w: e.activation(sq[0:64, 0:w], od[:, 0:w], AF.Square), reads=[od], writes=[sq])
                    ss = PC.next()
                    mm(ss, ss[0:64, 0:w], cmat, cmat[0:64, BLK64, 0:64], sq, sq[0:64, 0:w], True, True)
                    rstd = f32_p.next()
                    S.op("act", lambda e, rstd=rstd, ss=ss, w=w: e.activation(rstd[0:64, 0:w], ss[0:64, 0:w], AF.Sqrt, bias=EPS * 64, scale=1.0), reads=[ss], writes=[rstd])
                    S.op("dve", lambda e, rstd=rstd, ss=ss, w=w: e.reciprocal(rstd[0:64, 0:w], rstd[0:64, 0:w]), reads=[rstd], writes=[rstd])
                    on = on_p.next()
                    S.op("dve", lambda e, on=on, od=od, rstd=rstd, w=w: e.scalar_tensor_tensor(
                        on[:, 0:w], od[:, 0:w], gg[0:64, 27:28], rstd[0:64, 0:w], ALU.mult, ALU.mult),
                        reads=[od, gg, rstd], writes=[on])
                else:
                    on = onorm[0]
                dma_out(ot_b, ot_d[orow:orow + 64, t0:t0 + w], on, on[:, 0:w])

    def emit_merge(li, g, last):
        phase_start()
        hT = carve([128, 8, 1280], BF16)
        otg = Pool([carve([128, 4, 1280], BF16, dma=True) for _ in range(2)])
        yT = carve([128, 8, 1280], BF16)
        wo_p = Pool([carve([128, 4, 512], BF16, dma=True) for _ in range(2)])
        gt0, gt1 = group_range(g)
        blocks = [b for b in GROUPS[g] if not (last and b == 0)]
        ab = AB[li]
        for bi in blocks:
            emit_norm_mod(li, bi, 0, hT, TBS[bi][0] - gt0)
        wos = [woa_d[li], wob_d[li], woc_d[li]]
        for m in range(3):
            og = otg.next()
            dma_in(og, og[:, :, 0:gt1 - gt0],
                   ot_d[m * 512:(m + 1) * 512, gt0:gt1].rearrange("(c p) t -> p c t", p=128), extra_reads=[ot_b])
            for jg in range(2):
                gw = wchunk(winp_d[li], FMW + TMW + m * 1024 + jg * 512, 512)
                ow = wo_p.next()
                dma_in(ow, ow[:, :, :], wos[m][:, jg * 512:(jg + 1) * 512].rearrange("(c p) n -> p c n", p=128), q="pool")
                for bi in blocks:
                    t0, w = TBS[bi]
                    ho = t0 - gt0
                    for j4 in range(4):
                        j = jg * 4 + j4
                        pp = PA.next()
                        for kc_ in range(4):
                            mm(pp, pp[:, 0:w], ow, ow[:, kc_, j4 * 128:(j4 + 1) * 128], og, og[:, kc_, ho:ho + w], kc_ == 0, kc_ == 3)
                        gp = PB.next()
                        for dc in range(8):
                            mm(gp, gp[:, 0:w], gw, gw[:, dc, j4 * 128:(j4 + 1) * 128], hT, hT[:, dc, ho:ho + w], dc == 0, dc == 7)
                        sg = sig_p.next()
                        S.op("act", lambda e, sg=sg, gp=gp, w=w: e.activation(sg[:, 0:w], gp[:, 0:w], AF.Sigmoid), reads=[gp], writes=[sg])
                        if m == 0:
                            S.op("dve", lambda e, sg=sg, pp=pp, j=j, ho=ho, w=w: e.tensor_tensor(yT[:, j, ho:ho + w], pp[:, 0:w], sg[:, 0:w], ALU.mult),
                                 reads=[pp, sg], writes=[yT])
                        else:
                            tmp = f32_p.next()
                            S.op("dve", lambda e, tmp=tmp, sg=sg, pp=pp, w=w: e.tensor_tensor(tmp[:, 0:w], pp[:, 0:w], sg[:, 0:w], ALU.mult),
                                 reads=[pp, sg], writes=[tmp])
                            S.op("pool", lambda e, tmp=tmp, j=j, ho=ho, w=w: e.tensor_tensor(yT[:, j, ho:ho + w], yT[:, j, ho:ho + w], tmp[:, 0:w], ALU.add),
                                 reads=[tmp, yT], writes=[yT])
        for jg in range(2):
            ww = wchunk(wout_d[li], jg * 512, 512)
            for bi in blocks:
                t0, w = TBS[bi]
                ho = t0 - gt0
                cond = 1 if bi == 0 else 0
                for j4 in range(4):
                    j = jg * 4 + j4
                    zp = PA.next()
                    for dc in range(8):
                        mm(zp, zp[:, 0:w], ww, ww[:, dc, j4 * 128:(j4 + 1) * 128], yT, yT[:, dc, ho:ho + w], dc == 0, dc == 7)
                    x = xT[bi]
                    S.op("dve", lambda e, zp=zp, x=x, j=j, w=w, cond=cond: e.scalar_tensor_tensor(
                        x[:, j, :], zp[:, 0:w], ab[:, 2, j, cond:cond + 1], x[:, j, :], ALU.mult, ALU.add),
                        reads=[zp, ab, x], writes=[x])

    def emit_moe(li, last, n_exp=32):
        phase_start()
        h2 = [carve([128, 8, w], BF16) for (_, w) in TBS]
        o_act = uoff[0]
        act_all = carve([128, 4, NT], BF16)
        actT = [Buf(act_all[:, :, t0:t0 + w]) for (t0, w) in TBS]
        h2f = Buf(UF[:, o_act // 2:o_act // 2 + 4096].rearrange("p (c t) -> p c t", t=512))
        GT = carve([32, NT], BF16)
        GTm = Pool([carve([32, 512], BF16) for _ in range(2)])
        rw = carve([128, 8, 32], F32, dma=True)
        rb = carve([128, 32], F32, dma=True)
        bgu = carve([128, 32, 16], F32, dma=True)
        bgu1 = carve([128, 32, 8], F32)
        bdn = carve([32, 1024], BF16, dma=True)
        rt = carve([128, 4, 48], F32)
        gbc_p = Pool([carve([128, 512], BF16) for _ in range(2)])
        blocks = [b for b in range(5) if not (last and b == 0)]
        ab = AB[li]
        dma_in(rw, rw[:, :, :], rw_d[li])
        dma_in(rb, rb[:, :], rb_d[li])
        dma_in(bgu, bgu[:, :, :], bgu_d[li])
        dma_in(bdn, bdn[:, :], bdn_d[li], q="pool")
        S.op("dve", lambda e: e.tensor_scalar(bgu1[:, :, :], bgu[:, :, 8:16], 1.0, None, ALU.add), reads=[bgu], writes=[bgu1])
        for bi in blocks:
            t0, w = TBS[bi]
            emit_norm_mod(li, bi, 1, h2[bi], 0, f32_dst=h2f)
            for s_ in range(w // 128):
                lp = PC.next()
                for dc in range(8):
                    mm(lp, lp[:, 0:32], h2f, h2f[:, dc, s_ * 128:(s_ + 1) * 128], rw, rw[:, dc, :], dc == 0, dc == 7)
                r = rt
                S.op("dve", lambda e, lp=lp: e.tensor_tensor(r[:, 0, 0:32], lp[:, 0:32], rb[:, :], ALU.add), reads=[lp, rb], writes=[r])
                S.op("dve", lambda e: e.max(r[:, 1, 0:8], r[:, 0, 0:32]), reads=[r], writes=[r])
                S.op("dve", lambda e: e.tensor_scalar(r[:, 1, 8:9], r[:, 1, 0:1], -1.0, None, ALU.mult), reads=[r], writes=[r])
                S.op("dve", lambda e: e.tensor_scalar(r[:, 2, 0:32], r[:, 0, 0:32], r[:, 1, 3:4], None, ALU.is_ge), reads=[r], writes=[r])
                S.op("act", lambda e: e.activation(r[:, 3, 0:32], r[:, 0, 0:32], AF.Exp, bias=r[:, 1, 8:9]), reads=[r], writes=[r])
                S.op("dve", lambda e: e.tensor_tensor(r[:, 3, 0:32], r[:, 3, 0:32], r[:, 2, 0:32], ALU.mult), reads=[r], writes=[r])
                S.op("dve", lambda e: e.reduce_sum(r[:, 1, 9:10], r[:, 3, 0:32], AX.X), reads=[r], writes=[r])
                S.op("dve", lambda e: e.reciprocal(r[:, 1, 10:11], r[:, 1, 9:10]), reads=[r], writes=[r])
                S.op("dve", lambda e: e.tensor_scalar(r[:, 3, 0:32], r[:, 3, 0:32], r[:, 1, 10:11], None, ALU.mult), reads=[r], writes=[r])
                tp = PC.next()
                S.op("pe", lambda e, tp=tp: e.transpose(tp[0:32, 0:128], r[:, 3, 0:32], c32[:, 1, :]), reads=[r, c32], writes=[tp])
                S.op("act", lambda e, tp=tp, t0=t0, s_=s_: e.copy(GT[:, t0 + s_ * 128:t0 + (s_ + 1) * 128], tp[0:32, 0:128]), reads=[tp], writes=[GT])
        S.barrier()
        if dbg and dbg.get("dumpM"):
            dsm = S.new_sem(16)
            dd = nc.dram_tensor("d_GT", [32, NT], BF16, kind="ExternalOutput").ap()
            S.dma("sp", dd[:, :], GT[:, :], dsm, reads=[GT], writes=[])
            for bi_ in range(5):
                dd = nc.dram_tensor("d_h2_%d" % bi_, [128, 8, TBS[bi_][1]], BF16, kind="ExternalOutput").ap()
                S.dma("sp", dd[:, :, :], h2[bi_][:, :, :], dsm, reads=[h2[bi_]], writes=[])
            S.barrier()
        for bi in blocks:
            t0, w = TBS[bi]
            cond = 1 if bi == 0 else 0
            x = xT[bi]
            for j in range(8):
                bp = PC.next()
                mm(bp, bp[:, 0:w], bdn, bdn[:, j * 128:(j + 1) * 128], GT, GT[:, t0:t0 + w], True, True)
                S.op("dve", lambda e, bp=bp, x=x, j=j, w=w, cond=cond: e.scalar_tensor_tensor(
                    x[:, j, :], bp[:, 0:w], ab[:, 5, j, cond:cond + 1], x[:, j, :], ALU.mult, ALU.add),
                    reads=[bp, ab, x], writes=[x])
        for ex in range(n_exp):
            wg = wgu_d[li, ex]
            wd = wdn_d[li, ex]
            for half in range(2):
                gl = wchunk(wg, half * 512, 512)
                ln = wchunk(wg, 1024 + half * 512, 512)
                for bi in blocks:
                    t0, w = TBS[bi]
                    gm_ = GTm.next()
                    S.op("dve", lambda e, gm_=gm_, t0=t0, w=w, ex=ex: e.tensor_scalar(
                        gm_[:, 0:w], GT[:, t0:t0 + w], c32[0:32, 1, ex:ex + 1], None, ALU.mult), reads=[GT, c32], writes=[gm_])
                    gps = PC.next()
                    mm(gps, gps[:, 0:w], cmat, cmat[0:32, ONES, :], gm_, gm_[:, 0:w], True, True)
                    gb = gbc_p.next()
                    S.op("act", lambda e, gb=gb, gps=gps, w=w: e.copy(gb[:, 0:w], gps[:, 0:w]), reads=[gps], writes=[gb])
                    for f4 in range(4):
                        fc = half * 4 + f4
                        pg = PA.next()
                        pl = PB.next()
                        for dc in range(8):
                            mm(pg, pg[:, 0:w], gl, gl[:, dc, f4 * 128:(f4 + 1) * 128], h2[bi], h2[bi][:, dc, :], dc == 0, dc == 7)
                        for dc in range(8):
                            mm(pl, pl[:, 0:w], ln, ln[:, dc, f4 * 128:(f4 + 1) * 128], h2[bi], h2[bi][:, dc, :], dc == 0, dc == 7)
                        gv = f32_p.next()
                        S.op("dve", lambda e, gv=gv, pg=pg, fc=fc, w=w, ex=ex: e.tensor_scalar(
                            gv[:, 0:w], pg[:, 0:w], bgu[:, ex, fc:fc + 1], 7.0, ALU.add, ALU.min), reads=[pg, bgu], writes=[gv])
                        sg = sig_p.next()
                        S.op("act", lambda e, sg=sg, gv=gv, w=w: e.activation(sg[:, 0:w], gv[:, 0:w], AF.Sigmoid, scale=1.702),
                             reads=[gv], writes=[sg])
                        lv = f32_p.next()
                        S.op("dve", lambda e, lv=lv, pl=pl, fc=fc, w=w, ex=ex: e.tensor_scalar(
                            lv[:, 0:w], pl[:, 0:w], bgu1[:, ex, fc:fc + 1], -6.0, ALU.add, ALU.max), reads=[pl, bgu1], writes=[lv])
                        S.op("pool", lambda e, gv=gv, sg=sg, w=w: e.tensor_tensor(gv[:, 0:w], gv[:, 0:w], sg[:, 0:w], ALU.mult),
                             reads=[gv, sg], writes=[gv])
                        S.op("pool", lambda e, gv=gv, gb=gb, w=w: e.tensor_tensor(gv[:, 0:w], gv[:, 0:w], gb[:, 0:w], ALU.mult),
                             reads=[gv, gb], writes=[gv])
                        at = actT[bi]
                        S.op("dve", lambda e, at=at, gv=gv, lv=lv, f4=f4, w=w: e.scalar_tensor_tensor(
                            at[:, f4, :], lv[:, 0:w], 8.0, gv[:, 0:w], ALU.min, ALU.mult),
                             reads=[gv, lv], writes=[at])
                dwb = ring.next()
                dwv = RING_view(dwb)
                S.dma("pool", dwv, wd[half * 512:(half + 1) * 512, :].rearrange("(c p) n -> p c n", p=128), dwb.dsem, reads=[], writes=[dwb])
                for bi in blocks:
                    t0, w = TBS[bi]
                    cond = 1 if bi == 0 else 0
                    x = xT[bi]
                    at = actT[bi]
                    for j in range(8):
                        yp = PA.next()
                        for f4 in range(4):
                            mm(yp, yp[:, 0:w], dwb, dwv[:, f4, j * 128:(j + 1) * 128], at, at[:, f4, :], f4 == 0, f4 == 3)
                        S.op("dve", lambda e, yp=yp, x=x, j=j, w=w, cond=cond: e.scalar_tensor_tensor(
                            x[:, j, :], yp[:, 0:w], ab[:, 5, j, cond:cond + 1], x[:, j, :], ALU.mult, ALU.add),
                            reads=[yp, ab, x], writes=[x])

    def RING_view(b):
        return b.ap.rearrange("p c n -> p (c n)").rearrange("p (c n) -> p c n", n=1024)

    def emit_exchange():
        if mode != "fused":
            return
        E = S.E["pool"]
        waits = S._waits(E, [kvloc_b], [kvall_b])
        ccs = S.new_sem(16)
        ccs.n += 1
        E.prog.append((waits, lambda e: e.collective_compute(
            "AllGather", ALU.bypass, [[0, 1, 2, 3], [4, 5, 6, 7]],
            [kv_loc[:, :]], [kv_all[:, :]]), None, (ccs, 1)))
        kvloc_b.r[ccs] = 1
        kvall_b.w = (ccs, 1)
        kvall_b.r = {}

    stop = dbg.get("stop") if dbg else None
    for li in range(nl):
        emit_modulation(li)
    for li in range(nl):
        last = (layers[li] == L - 1)
        emit_phase_P(li, 0)
        emit_phase_P(li, 1)
        if mode == "A" or stop == "P":
            break
        emit_exchange()
        emit_attention(li, last, heads=dbg.get("heads") if dbg else None)
        if stop == "attn":
            break
        emit_merge(li, 0, last)
        emit_merge(li, 1, last)
        if stop == "merge":
            break
        emit_moe(li, last, n_exp=dbg.get("n_exp", 32) if dbg else 32)
    S.barrier()
    if mode == "A":
        S.finish("sp", [kvloc_b])
    else:
        outb = Buf(out_d)
        osem = S.new_sem(16)
        for bi, (t0, w) in enumerate(TBS):
            S.dma("sp", out_d[:, :, t0:t0 + w], xT[bi][:, :, :], osem, reads=[xT[bi]], writes=[outb])
        S.finish("sp", [outb])
    S.emit()
    return nc


def _rope_tables(rot_dim, n_lat):
    n_rows = n_lat // 64
    row = np.repeat(np.arange(n_rows, dtype=np.float32), 64)
    col = np.tile(np.arange(64, dtype=np.float32), n_rows)
    axis_pairs = rot_dim // 4
    inv = (10000.0 ** (-np.arange(axis_pairs, dtype=np.float32) / axis_pairs)).astype(np.float32)
    ang = np.concatenate([row[:, None] * inv, col[:, None] * inv], axis=-1).astype(np.float32)
    return np.cos(ang).astype(np.float32), np.sin(ang).astype(np.float32)


def _const_mats():
    m = np.zeros((128, 6, 128), np.float32)
    m[:, 0, :] = 1.0
    for i in range(128):
        for j in range(128):
            if i // 64 == j // 64:
                m[i, 1, j] = 1.0
            if i // 32 == j // 32:
                m[i, 2, j] = 1.0
    for (slot, blk) in ((3, 64), (4, 32)):
        hh = blk // 2
        for mm_ in range(128):
            i = mm_ % blk
            base = mm_ - i
            if i < hh:
                m[base + i + hh, slot, mm_] = -1.0
            else:
                m[base + i - hh, slot, mm_] = 1.0
    m[:, 5, :] = np.eye(128, dtype=np.float32)
    return m


def _feat_major(v):
    return np.ascontiguousarray(v.reshape(8, 128).T)


def _prep_shared(inp, layers):
    f = np.float32
    out = {}
    ls = list(layers)
    sel = np.zeros((32, 32, 128), f)
    for e in range(32):
        sel[e, e, :] = 1.0
    out["sel"] = sel
    out["cmat"] = _const_mats()
    gm = np.zeros((len(ls), 128, NG), f)
    gains = np.zeros((len(ls), 128, NG), f)
    for i, l in enumerate(ls):
        lam_init = 0.8 - 0.6 * np.exp(-0.3 * l)
        gm[i, :, 0:16] = 32.0
        gm[i, :, 16] = 8.0; gm[i, :, 17] = 8.0
        gm[i, :, 18:20] = 16.0
        gm[i, :, 20] = np.sqrt(128.0)
        gm[i, :, 21] = 8.0; gm[i, :, 22] = np.sqrt(32.0); gm[i, :, 23] = 8.0; gm[i, :, 24] = np.sqrt(32.0)
        gm[i, :, 25] = np.sqrt(32.0); gm[i, :, 26] = np.sqrt(32.0)
        gm[i, :, 27] = 8.0 * (1.0 - lam_init)
        gains[i, :, 0:8] = _feat_major(inp["norm_mix"][l])
        gains[i, :, 8:16] = _feat_major(inp["norm_ffn"][l])
        gains[i, :, 16] = np.tile(inp["a_q_norm"][l], 2)
        gains[i, :, 17] = np.tile(inp["a_k_norm"][l], 2)
        gains[i, :, 18:20] = inp["b_q_a_norm"][l].reshape(2, 128).T
        gains[i, :, 20] = inp["b_kv_a_norm"][l]
        gains[i, :, 21] = np.tile(inp["b_q_norm"][l][:64], 2)
        gains[i, :, 22] = np.tile(inp["b_q_norm"][l][64:], 4)
        gains[i, :, 23] = np.tile(inp["b_k_norm"][l][:64], 2)
        gains[i, :, 24] = np.tile(inp["b_k_norm"][l][64:], 4)
        gains[i, :, 25] = np.tile(inp["c_q_norm"][l], 4)
        gains[i, :, 26] = np.tile(inp["c_k_norm"][l], 4)
        gains[i, :, 27] = np.tile(inp["c_subln"][l], 2)
    out["gmul"] = gm
    out["gains"] = gains
    out["clam"] = np.ascontiguousarray(np.broadcast_to(inp["c_lambda"][ls].reshape(len(ls), 1, 128), (len(ls), 128, 128))).astype(f)
    li_ = np.array([0.8 - 0.6 * np.exp(-0.3 * l) for l in ls], f)
    out["laminit"] = np.ascontiguousarray(np.broadcast_to(li_.reshape(-1, 1, 1), (len(ls), 128, 1))).astype(f)
    out["bmod"] = np.ascontiguousarray(inp["b_mod"][ls].reshape(len(ls), 48, 128).transpose(0, 2, 1))
    out["rb"] = np.ascontiguousarray(np.broadcast_to(inp["router_b"][ls][:, None, :], (len(ls), 128, 32))).astype(f)
    out["bgu"] = np.ascontiguousarray(inp["exp_b_gu"][ls].reshape(len(ls), 32, 16, 128).transpose(0, 3, 1, 2))
    out["bdn"] = np.ascontiguousarray(inp["exp_b_down"][ls])
    out["rw"] = np.ascontiguousarray(inp["router_w"][ls].reshape(len(ls), 8, 128, 32).transpose(0, 2, 1, 3))
    out["wmod"] = np.ascontiguousarray(inp["w_mod"][ls])
    w_in = inp["w_in"][ls]
    sp = np.cumsum([0, 512, 128, 128, 256, 128, 32, 512, 512, 512, 3072])
    a_q, a_k, a_v, b_cq, b_ckv, b_kr, c_q, c_k, c_v, gates = [w_in[:, :, sp[i]:sp[i + 1]] for i in range(10)]
    pad = np.zeros((len(ls), 1024, 96), f)
    out["winp"] = np.ascontiguousarray(np.concatenate([a_q, a_k, b_cq, b_ckv, c_q, c_k, b_kr, pad, a_v, c_v, gates], axis=2))
    uq = inp["b_w_uq"][ls].reshape(len(ls), 256, 8, 96)
    out["wuq"] = np.ascontiguousarray(np.concatenate([uq[..., :64].reshape(len(ls), 256, 512), uq[..., 64:].reshape(len(ls), 256, 256)], axis=2))
    ukv = inp["b_w_ukv"][ls].reshape(len(ls), 128, 8, 128)
    out["wukv"] = np.ascontiguousarray(np.concatenate([ukv[..., :64].reshape(len(ls), 128, 512), ukv[..., 64:].reshape(len(ls), 128, 512)], axis=2))
    out["woa"] = np.ascontiguousarray(inp["w_o_a"][ls]); out["wob"] = np.ascontiguousarray(inp["w_o_b"][ls]); out["woc"] = np.ascontiguousarray(inp["w_o_c"][ls])
    out["wout"] = np.ascontiguousarray(inp["w_out"][ls])
    out["wgu"] = np.ascontiguousarray(inp["exp_w_gu"][ls])
    out["wdn"] = np.ascontiguousarray(inp["exp_w_down"][ls])
    return out


def _prep_core(inp, core, x_lat, x_ctx):
    b, r = core // 4, core % 4
    f = np.float32
    xt = np.concatenate([x_ctx[b], x_lat[b, r * NLAT:(r + 1) * NLAT]], axis=0)
    xT = np.ascontiguousarray(xt.reshape(NT, 8, 128).transpose(2, 1, 0)).astype(f)
    cond = np.stack([inp["c"][b], inp["c_ctx"]], axis=-1)
    condT = np.ascontiguousarray(cond.reshape(8, 128, 2).transpose(1, 0, 2)).astype(f)
    out = {"xT_in": xT, "condT": condT}
    for nm, rd, blk in (("A", 64, 64), ("B", 32, 32)):
        cs, sn = _rope_tables(rd, 8192)
        cs = cs[r * NLAT:(r + 1) * NLAT]; sn = sn[r * NLAT:(r + 1) * NLAT]
        idx = (np.arange(128) % blk) % (rd // 2)
        out["cos" + nm] = np.ascontiguousarray(cs[:, idx].T).astype(f)
        out["sin" + nm] = np.ascontiguousarray(sn[:, idx].T).astype(f)
    return out


def _unpack_x(res_list):
    x_lat = np.zeros((2, 8192, 1024), np.float32)
    x_ctx = np.zeros((2, 256, 1024), np.float32)
    for core, o in enumerate(res_list):
        b, r = core // 4, core % 4
        xt = np.asarray(o).transpose(2, 1, 0).reshape(NT, 1024)
        x_lat[b, r * NLAT:(r + 1) * NLAT] = xt[NCTX:]
        if r == 0:
            x_ctx[b] = xt[:NCTX]
    return x_lat, x_ctx


A_KEYS = ("xT_in", "condT", "cosA", "sinA", "cosB", "sinB", "cmat", "gmul", "gains", "clam", "laminit", "bmod",
          "wmod", "winp", "wuq", "wukv")


def _filter(m, mode):
    if mode == "A":
        return {k: v for k, v in m.items() if k in A_KEYS}
    return {k: v for k, v in m.items() if k != "sel"}


_CACHE = {}


def _get_prog(mode, last):
    key = (mode, bool(last))
    if key not in _CACHE:
        _CACHE[key] = build_program(mode, [L - 1 if last else 0])
    return _CACHE[key]


def kernel(**inp):
    inp = {k: np.asarray(v) for k, v in inp.items()}
    x_lat = np.ascontiguousarray(inp["x"], dtype=np.float32)
    x_ctx = np.ascontiguousarray(inp["ctx"], dtype=np.float32)
    for l in range(L):
        last = (l == L - 1)
        shared = _prep_shared(inp, [l])
        maps = []
        for core in range(8):
            m = dict(shared)
            m.update(_prep_core(inp, core, x_lat, x_ctx))
            maps.append(m)
        resA = run_bass_kernel_spmd(_get_prog("A", False), [_filter(m, "A") for m in maps], core_ids=list(range(8)))
        kv = [np.asarray(r["kv_loc"]) for r in resA.results]
        for core in range(8):
            b = core // 4
            maps[core]["kv_all"] = np.ascontiguousarray(np.concatenate(kv[4 * b:4 * b + 4], axis=0))
        resB = run_bass_kernel_spmd(_get_prog("B", last), [_filter(m, "B") for m in maps], core_ids=list(range(8)))
        x_lat, x_ctx_new = _unpack_x([r["out"] for r in resB.results])
        if not last:
            x_ctx = x_ctx_new
    return x_lat.astype(np.float32)
```

```python
import numpy as np
import concourse.bass as bass
import concourse.mybir as mybir
from concourse.bass_utils import run_bass_kernel_spmd

F32 = mybir.dt.float32
BF16 = mybir.dt.bfloat16
ALU = mybir.AluOpType
AF = mybir.ActivationFunctionType
AX = mybir.AxisListType

L = 4
D = 1024
NLAT = 2048
NCTX = 256
NT = NCTX + NLAT
TBS = [(0, 256), (256, 512), (768, 512), (1280, 512), (1792, 512)]
GROUPS = [[0, 1, 2], [3, 4]]
NKT = 66
NKEY = NKT * 128
EPS = 1e-6
KT_ROWS = 1184
V_COLS = 1152
KV_ROWS = KT_ROWS + V_COLS
QT_ROWS = 1792
OT_ROWS = 1536
NG = 28
FMW = 2176
TMW = 640
WINP = FMW + TMW + 3072


class Sem:
    def __init__(self, h, step):
        self.h = h
        self.step = step
        self.n = 0
        self.signal = None


class Eng:
    def __init__(self, name, sem):
        self.name = name
        self.sem = sem
        self.prog = []
        self.seen = {}
        self.sig = set()


class Buf:
    def __init__(self, ap, dsem=None):
        self.ap = ap
        self.w = None
        self.r = {}
        self.dsem = dsem

    def __getitem__(self, k):
        return self.ap[k]


class Sched:
    def __init__(self, nc):
        self.nc = nc
        self.sems_used = 0
        self.dsems = []
        self.E = {}
        for name in ("pe", "act", "dve", "pool", "sp"):
            self.E[name] = Eng(name, self.new_sem(1))

    def new_sem(self, step):
        self.sems_used += 1
        sm = Sem(self.nc.alloc_semaphore(name=f"s{self.sems_used}"), step)
        if step == 16:
            self.dsems.append(sm)
        return sm

    def barrier(self):
        for E in self.E.values():
            waits = []
            for E2 in self.E.values():
                if E2 is E or E2.sem.n == 0:
                    continue
                if E.seen.get(E2.sem, 0) < E2.sem.n:
                    E.seen[E2.sem] = E2.sem.n
                    waits.append((E2.sem, E2.sem.n))
                    E2.sig.add(E2.sem.n)
            for sm in self.dsems:
                if sm.n > 0 and E.seen.get(sm, 0) < sm.n:
                    E.seen[sm] = sm.n
                    waits.append((sm, sm.n))
            E.prog.append((waits, None, None, None))

    def _waits(self, E, reads, writes):
        deps = {}

        def add(t):
            if t is None:
                return
            s, n = t
            if s is E.sem and E.name in ("pe", "sp"):
                return
            if deps.get(s, 0) < n:
                deps[s] = n

        for b in reads:
            add(b.w)
        for b in writes:
            add(b.w)
            for s, n in b.r.items():
                add((s, n))
        waits = []
        for s, n in deps.items():
            if E.seen.get(s, 0) < n:
                E.seen[s] = n
                waits.append((s, n))
                if s.step == 1:
                    s.owner.sig.add(n)
        return waits

    def op(self, ename, fn, reads=(), writes=()):
        E = self.E[ename]
        waits = self._waits(E, reads, writes)
        E.sem.n += 1
        n = E.sem.n
        E.prog.append((waits, fn, n, None))
        for b in reads:
            b.r[E.sem] = n
        for b in writes:
            b.w = (E.sem, n)
            b.r = {}

    def dma(self, ename, out_ap, in_ap, sem, reads=(), writes=()):
        E = self.E[ename]
        waits = self._waits(E, reads, writes)
        sem.n += 1
        n = sem.n
        E.prog.append((waits, lambda e: e.dma_start(out=out_ap, in_=in_ap), None, (sem, n)))
        for b in reads:
            b.r[sem] = n
        for b in writes:
            b.w = (sem, n)
            b.r = {}

    def finish(self, ename, bufs):
        E = self.E[ename]
        waits = self._waits(E, bufs, [])
        E.prog.append((waits, None, None, None))

    def emit(self):
        for E in self.E.values():
            E.sem.owner = E
        for E in self.E.values():
            E.sigl = sorted(E.sig)
            E.rank = {n: i + 1 for i, n in enumerate(E.sigl)}

        def val(s, n):
            if s.step == 16:
                return 16 * n
            return s.owner.rank[n]

        def run(E, e):
            for waits, fn, n, dm in E.prog:
                for s, nn in waits:
                    e.wait_ge(s.h, val(s, nn))
                if fn is None:
                    continue
                ins = fn(e)
                if dm is not None:
                    ins.then_inc(dm[0].h, 16)
                elif n in E.sig:
                    ins.then_inc(E.sem.h, 1)

        with self.nc.Block() as block:
            @block.tensor
            def _(e):
                run(self.E["pe"], e)

            @block.scalar
            def _(e):
                run(self.E["act"], e)

            @block.vector
            def _(e):
                run(self.E["dve"], e)

            @block.gpsimd
            def _(e):
                run(self.E["pool"], e)

            @block.sync
            def _(e):
                run(self.E["sp"], e)


class Pool:
    def __init__(self, bufs):
        self.bufs = bufs
        self.i = 0

    def next(self):
        b = self.bufs[self.i % len(self.bufs)]
        self.i += 1
        return b


def build_program(mode, layers, n_ring=4, dbg=None):
    nc = bass.Bass("TRN2", target_bir_lowering=False)
    S = Sched(nc)
    for E in S.E.values():
        E.sem.owner = E
    nl = len(layers)

    def din(name, shape, dt=F32):
        return nc.dram_tensor(name, list(shape), dt, kind="ExternalInput").ap()

    def dout(name, shape, dt=F32):
        return nc.dram_tensor(name, list(shape), dt, kind="ExternalOutput").ap()

    def dint(name, shape, dt=BF16):
        if dbg:
            return nc.dram_tensor(name, list(shape), dt, kind="ExternalOutput").ap()
        return nc.dram_tensor(name, list(shape), dt, kind="Internal").ap()

    xT_in = din("xT_in", [128, 8, NT])
    condT = din("condT", [128, 8, 2])
    cosA_d = din("cosA", [128, NLAT]); sinA_d = din("sinA", [128, NLAT])
    cosB_d = din("cosB", [128, NLAT]); sinB_d = din("sinB", [128, NLAT])
    cmat_d = din("cmat", [128, 6, 128])
    gmul_d = din("gmul", [nl, 128, NG])
    gains_d = din("gains", [nl, 128, NG])
    clam_d = din("clam", [nl, 128, 128])
    laminit_d = din("laminit", [nl, 128, 1])
    bmod_d = din("bmod", [nl, 128, 48])
    wmod_d = din("wmod", [nl, 1024, 6144])
    winp_d = din("winp", [nl, 1024, WINP])
    wuq_d = din("wuq", [nl, 256, 768])
    wukv_d = din("wukv", [nl, 128, 1024])
    if mode != "A":
        rb_d = din("rb", [nl, 128, 32])
        bgu_d = din("bgu", [nl, 128, 32, 16])
        bdn_d = din("bdn", [nl, 32, 1024])
        rw_d = din("rw", [nl, 128, 8, 32])
        woa_d = din("woa", [nl, 512, 1024]); wob_d = din("wob", [nl, 512, 1024]); woc_d = din("woc", [nl, 512, 1024])
        wout_d = din("wout", [nl, 1024, 1024])
        wgu_d = din("wgu", [nl, 32, 1024, 2048])
        wdn_d = din("wdn", [nl, 32, 1024, 1024])

    if mode == "A":
        kv_loc = dout("kv_loc", [KV_ROWS, NLAT], BF16)
    else:
        kv_loc = dint("kv_loc", [KV_ROWS, NLAT])
    if mode == "B":
        kv_all = din("kv_all", [4 * KV_ROWS, NLAT], BF16)
    elif mode == "fused":
        kv_all = nc.dram_tensor("kv_all", [4 * KV_ROWS, NLAT], BF16, kind="Internal").ap()
    else:
        kv_all = None
    ktc_d = dint("ktc", [KT_ROWS, NCTX])
    vc_d = dint("vc", [NCTX, V_COLS])
    qt_d = dint("qt", [QT_ROWS, NT])
    ot_d = dint("ot", [OT_ROWS, NT])
    if mode != "A":
        out_d = dout("out", [128, 8, NT])

    kvloc_b = Buf(kv_loc); kvall_b = Buf(kv_all); ktc_b = Buf(ktc_d); vc_b = Buf(vc_d)
    qt_b = Buf(qt_d); ot_b = Buf(ot_d)

    cnt = [0]

    def sb(shape, dt, dma=False):
        cnt[0] += 1
        h = nc.alloc_sbuf_tensor(f"t{cnt[0]}", list(shape), dt)
        return Buf(h, S.new_sem(16) if dma else None)

    def sbpool(n, shape, dt, dma=False):
        return Pool([sb(shape, dt, dma) for _ in range(n)])

    USIZE = 36352
    phase_sems = []
    ps_idx = [0]

    def psem():
        if ps_idx[0] == len(phase_sems):
            phase_sems.append(S.new_sem(16))
        sm = phase_sems[ps_idx[0]]
        ps_idx[0] += 1
        return sm

    def phase_start():
        S.barrier()
        uoff[0] = 0
        ps_idx[0] = 0

    U = nc.alloc_sbuf_tensor("union", [128, USIZE], BF16)
    UF = U[:, :].bitcast(F32)
    uoff = [0]

    def carve(shape, dt, dma=False):
        per = 1
        for d_ in shape[1:]:
            per *= d_
        cols = per * (2 if dt == F32 else 1)
        cols_al = (cols + 15) // 16 * 16
        o = uoff[0]
        assert o + cols_al <= USIZE, ("union overflow", o, cols_al)
        uoff[0] = o + cols_al
        if dt == F32:
            ap = UF[0:shape[0], o // 2:(o + cols) // 2]
        else:
            ap = U[0:shape[0], o:o + cols]
        if len(shape) == 3:
            ap = ap.rearrange("p (a b) -> p a b", b=shape[2])
        elif len(shape) == 4:
            ap = ap.rearrange("p (a b c) -> p a b c", b=shape[2], c=shape[3])
        return Buf(ap, psem() if dma else None)

    banks = []
    for i in range(8):
        banks.append(Buf(nc.alloc_psum_tensor(f"ps{i}", [128, 512], F32)))
    PA = Pool(banks[0:3]); PB = Pool(banks[3:5]); PC = Pool(banks[5:7]); PD = Pool(banks[7:8])

    xT = [sb([128, 8, w], F32, dma=True) for (_, w) in TBS]
    cmat = sb([128, 6, 128], BF16, dma=True)
    c32 = sb([128, 2, 128], F32, dma=True)
    ONES, BLK64, BLK32, R64, R32, IDENT = range(6)
    silu_c = sb([128, 8, 2], BF16)
    cond32 = sb([128, 8, 2], F32, dma=True)
    modT = [sb([128, 48, 2], F32) for _ in layers]
    gains = [sb([128, NG], F32, dma=True) for _ in layers]
    gmul = sb([128, NG], F32, dma=True)
    bmod = sb([128, 48], F32, dma=True)
    AB = [sb([128, 6, 8, 2], F32) for _ in layers]
    neglam = [sb([128, 1], F32) for _ in layers]
    clam = sb([128, 128], F32, dma=True)
    laminit = sb([128, 1], F32, dma=True)
    lamtmp = sb([128, 68], F32)

    RING = nc.alloc_sbuf_tensor("ring", [128, n_ring * 4096], BF16)
    ring = Pool([Buf(RING[:, i * 4096:(i + 1) * 4096].rearrange("p (c n) -> p c n", n=512), S.new_sem(16)) for i in range(n_ring)])
    sq_p = sbpool(2, [128, 512], BF16)
    f32_p = sbpool(5, [128, 512], F32)
    sig_p = sbpool(2, [128, 512], F32)

    def dma_in(buf, dst_ap, src_ap, q="sp", extra_reads=()):
        S.dma(q, dst_ap, src_ap, buf.dsem, reads=list(extra_reads), writes=[buf])

    def dma_out(dst_buf, dst_ap, src_buf, src_ap, q="sp"):
        S.dma(q, dst_ap, src_ap, src_buf.dsem, reads=[src_buf], writes=[dst_buf])

    def mm(ps, ps_ap, lhsT_buf, lhsT_ap, rhs_buf, rhs_ap, start, stop):
        S.op("pe", lambda e: e.matmul(ps_ap, lhsT_ap, rhs_ap, start=start, stop=stop),
             reads=[lhsT_buf, rhs_buf], writes=[ps])

    for bi, (t0, w) in enumerate(TBS):
        dma_in(xT[bi], xT[bi][:, :, :], xT_in[:, :, t0:t0 + w])
    dma_in(cmat, cmat[:, :, :], cmat_d[:, :, :], q="pool")
    dma_in(c32, c32[:, 0, :], cmat_d[:, 0, :])
    dma_in(c32, c32[:, 1, :], cmat_d[:, 5, :])
    dma_in(cond32, cond32[:, :, :], condT[:, :, :])
    S.op("act", lambda e: e.activation(silu_c[:, :, :], cond32[:, :, :], AF.Silu), reads=[cond32], writes=[silu_c])

    def wchunk(w2d, c0, ncols, krows=1024):
        kc = krows // 128
        src = w2d[:, c0:c0 + ncols].rearrange("(c p) n -> p c n", p=128)
        b = ring.next()
        S.dma("pool", b[:, 0:kc, 0:ncols], src, b.dsem, reads=[], writes=[b])
        return b

    def emit_modulation(li):
        l_w = wmod_d[li]
        dma_in(gains[li], gains[li][:, :], gains_d[li])
        dma_in(gmul, gmul[:, :], gmul_d[li])
        dma_in(bmod, bmod[:, :], bmod_d[li])
        ps = PD.next()
        for cc in range(12):
            wb = wchunk(l_w, cc * 512, 512)
            for j4 in range(4):
                j = cc * 4 + j4
                for dc in range(8):
                    mm(ps, ps[:, 2 * j:2 * j + 2], wb, wb[:, dc, j4 * 128:(j4 + 1) * 128],
                       silu_c, silu_c[:, dc, :], dc == 0, dc == 7)
        m = modT[li]
        S.op("dve", lambda e: e.tensor_tensor(
            m[:, :, :], ps[:, 0:96].rearrange("p (j k) -> p j k", k=2),
            bmod[:, :].unsqueeze(2).to_broadcast([128, 48, 2]), ALU.add),
            reads=[ps, bmod], writes=[m])
        g = gains[li]
        S.op("dve", lambda e: e.tensor_tensor(g[:, :], g[:, :], gmul[:, :], ALU.mult),
             reads=[g, gmul], writes=[g])
        ab = AB[li]
        for k, (sc_j, sh_j, g_j, gcol) in enumerate([(8, 0, 16, 0), (32, 24, 40, 8)]):
            S.op("dve", lambda e, k=k, sc_j=sc_j, gcol=gcol: e.scalar_tensor_tensor(
                ab[:, 3 * k + 0, :, :], m[:, sc_j:sc_j + 8, :], 1.0,
                g[:, gcol:gcol + 8].unsqueeze(2).to_broadcast([128, 8, 2]), ALU.add, ALU.mult),
                reads=[m, g], writes=[ab])
            S.op("dve", lambda e, k=k, sh_j=sh_j: e.tensor_copy(ab[:, 3 * k + 1, :, :], m[:, sh_j:sh_j + 8, :]),
                 reads=[m], writes=[ab])
            S.op("dve", lambda e, k=k, g_j=g_j: e.tensor_copy(ab[:, 3 * k + 2, :, :], m[:, g_j:g_j + 8, :]),
                 reads=[m], writes=[ab])
        dma_in(clam, clam[:, :], clam_d[li])
        dma_in(laminit, laminit[:, :], laminit_d[li])
        lt = lamtmp
        S.op("dve", lambda e: e.tensor_tensor(lt[:, 0:32], clam[:, 0:32], clam[:, 32:64], ALU.mult), reads=[clam], writes=[lt])
        S.op("dve", lambda e: e.tensor_tensor(lt[:, 32:64], clam[:, 64:96], clam[:, 96:128], ALU.mult), reads=[clam], writes=[lt])
        S.op("dve", lambda e: e.reduce_sum(lt[:, 64:65], lt[:, 0:32], AX.X), reads=[lt], writes=[lt])
        S.op("dve", lambda e: e.reduce_sum(lt[:, 65:66], lt[:, 32:64], AX.X), reads=[lt], writes=[lt])
        S.op("act", lambda e: e.activation(lt[:, 66:68], lt[:, 64:66], AF.Exp), reads=[lt], writes=[lt])
        nlm = neglam[li]
        S.op("dve", lambda e: e.tensor_tensor(nlm[:, :], lt[:, 67:68], lt[:, 66:67], ALU.subtract), reads=[lt], writes=[nlm])
        S.op("dve", lambda e: e.tensor_tensor(nlm[:, :], nlm[:, :], laminit[:, :], ALU.subtract), reads=[nlm, laminit], writes=[nlm])

    def emit_norm_mod(li, bi, which, dst, dst_off, f32_dst=None, f32_deps=()):
        t0, w = TBS[bi]
        cond = 1 if bi == 0 else 0
        ab = AB[li]
        x = xT[bi]
        ps = PD.next()
        for dc in range(8):
            sq = sq_p.next()
            S.op("act", lambda e, sq=sq, dc=dc: e.activation(sq[:, 0:w], x[:, dc, :], AF.Square), reads=[x], writes=[sq])
            mm(ps, ps[:, 0:w], cmat, cmat[:, ONES, :], sq, sq[:, 0:w], dc == 0, dc == 7)
        rstd = f32_p.next()
        S.op("act", lambda e: e.activation(rstd[:, 0:w], ps[:, 0:w], AF.Sqrt, bias=EPS * D, scale=1.0), reads=[ps], writes=[rstd])
        S.op("dve", lambda e: e.reciprocal(rstd[:, 0:w], rstd[:, 0:w]), reads=[rstd], writes=[rstd])
        fd = list(f32_deps)
        for dc in range(8):
            tmp = sig_p.next()
            S.op("dve", lambda e, tmp=tmp, dc=dc: e.scalar_tensor_tensor(
                tmp[:, 0:w], x[:, dc, :], ab[:, 3 * which, dc, cond:cond + 1], rstd[:, 0:w], ALU.mult, ALU.mult),
                reads=[x, ab, rstd], writes=[tmp])
            if f32_dst is not None:
                S.op("act", lambda e, tmp=tmp, dc=dc: e.activation(
                    f32_dst[:, dc, 0:w], tmp[:, 0:w], AF.Identity, bias=ab[:, 3 * which + 1, dc, cond:cond + 1]),
                    reads=[tmp, ab], writes=[f32_dst] + fd)
                S.op("pool", lambda e, dc=dc: e.tensor_copy(dst[:, dc, dst_off:dst_off + w], f32_dst[:, dc, 0:w]),
                     reads=[f32_dst] + fd, writes=[dst])
            else:
                S.op("act", lambda e, tmp=tmp, dc=dc: e.activation(
                    dst[:, dc, dst_off:dst_off + w], tmp[:, 0:w], AF.Identity, bias=ab[:, 3 * which + 1, dc, cond:cond + 1]),
                    reads=[tmp, ab], writes=[dst])

    def group_range(g):
        t0 = TBS[GROUPS[g][0]][0]
        t1 = TBS[GROUPS[g][-1]][0] + TBS[GROUPS[g][-1]][1]
        return t0, t1

    def emit_phase_P(li, g):
        phase_start()
        bf_p = Pool([carve([128, 512], BF16, dma=True) for _ in range(3)])
        st_p = Pool([carve([128, 512], BF16, dma=True) for _ in range(3)])
        hT = carve([128, 8, 1280], BF16)
        cosA = carve([128, 1024], F32, dma=True); sinA = carve([128, 1024], F32, dma=True)
        cosB = carve([128, 1024], F32, dma=True); sinB = carve([128, 1024], F32, dma=True)
        cqn = carve([128, 2, 1280], BF16)
        ckvn = carve([128, 1280], BF16)
        wuq = carve([128, 2, 768], BF16, dma=True)
        wukv = carve([128, 1024], BF16, dma=True)
        gg = gains[li]

        gt0, gt1 = group_range(g)
        blocks = GROUPS[g]
        lat0 = max(gt0, NCTX) - NCTX
        latn = gt1 - NCTX - lat0
        for tb_, src in ((cosA, cosA_d), (sinA, sinA_d), (cosB, cosB_d), (sinB, sinB_d)):
            dma_in(tb_, tb_[:, 0:latn], src[:, lat0:lat0 + latn])
        dma_in(wuq, wuq[:, :, :], wuq_d[li].rearrange("(c p) n -> p c n", p=128), q="pool")
        dma_in(wukv, wukv[:, :], wukv_d[li], q="pool")
        for bi in blocks:
            emit_norm_mod(li, bi, 0, hT, TBS[bi][0] - gt0)
        w_l = winp_d[li]
        ropeA = (R64, cosA, sinA)
        ropeB = (R32, cosB, sinB)

        def fm_finish(raw_ps, rows, w, blk, dim, gcol, rope, lat_off, dsts, is_ctx):
            sq = sq_p.next()
            S.op("act", lambda e: e.activation(sq[0:rows, 0:w], raw_ps[0:rows, 0:w], AF.Square), reads=[raw_ps], writes=[sq])
            ss = PB.next()
            mm(ss, ss[0:rows, 0:w], cmat, cmat[0:rows, blk, 0:rows], sq, sq[0:rows, 0:w], True, True)
            rstd = f32_p.next()
            S.op("act", lambda e: e.activation(rstd[0:rows, 0:w], ss[0:rows, 0:w], AF.Sqrt, bias=EPS * dim, scale=1.0), reads=[ss], writes=[rstd])
            S.op("dve", lambda e: e.reciprocal(rstd[0:rows, 0:w], rstd[0:rows, 0:w]), reads=[rstd], writes=[rstd])
            qn = bf_p.next()
            S.op("dve", lambda e: e.scalar_tensor_tensor(qn[0:rows, 0:w], raw_ps[0:rows, 0:w], gg[0:rows, gcol:gcol + 1],
                                                        rstd[0:rows, 0:w], ALU.mult, ALU.mult),
                 reads=[raw_ps, gg, rstd], writes=[qn])
            if rope is not None and not is_ctx:
                rmat, cs, sn = rope
                rot = PC.next()
                mm(rot, rot[0:rows, 0:w], cmat, cmat[0:rows, rmat, 0:rows], qn, qn[0:rows, 0:w], True, True)
                t1 = f32_p.next()
                S.op("pool", lambda e: e.tensor_tensor(t1[0:rows, 0:w], qn[0:rows, 0:w], cs[0:rows, lat_off:lat_off + w], ALU.mult),
                     reads=[qn, cs], writes=[t1])
                t2 = f32_p.next()
                S.op("dve", lambda e: e.tensor_tensor(t2[0:rows, 0:w], rot[0:rows, 0:w], sn[0:rows, lat_off:lat_off + w], ALU.mult),
                     reads=[rot, sn], writes=[t2])
                ob = st_p.next()
                S.op("pool", lambda e: e.tensor_tensor(ob[0:rows, 0:w], t1[0:rows, 0:w], t2[0:rows, 0:w], ALU.add),
                     reads=[t1, t2], writes=[ob])
            else:
                ob = qn
            for (r0, r1, dbuf, dap) in dsts:
                dma_out(dbuf, dap, ob, ob[r0:r1, 0:w])

        def kdst(bi, r0, nrows, krow0):
            t0, w = TBS[bi]
            if bi == 0:
                return (r0, r0 + nrows, ktc_b, ktc_d[krow0:krow0 + nrows, 0:w])
            return (r0, r0 + nrows, kvloc_b, kv_loc[krow0:krow0 + nrows, t0 - NCTX:t0 - NCTX + w])

        def qdst(bi, r0, nrows, qrow0):
            t0, w = TBS[bi]
            return (r0, r0 + nrows, qt_b, qt_d[qrow0:qrow0 + nrows, t0:t0 + w])

        for rc0 in range(0, 17, 4):
            nrc = min(4, 17 - rc0)
            wb = wchunk(w_l, rc0 * 128, nrc * 128)
            for bi in blocks:
                t0, w = TBS[bi]
                ho = t0 - gt0
                lo = t0 - NCTX - lat0
                ctxb = (bi == 0)
                raw5 = None
                for r in range(nrc):
                    idx = rc0 + r
                    raw = PA.next()
                    for dc in range(8):
                        mm(raw, raw[:, 0:w], wb, wb[:, dc, r * 128:(r + 1) * 128], hT, hT[:, dc, ho:ho + w], dc == 0, dc == 7)
                    if idx < 4:
                        fm_finish(raw, 128, w, BLK64, 64, 16, ropeA, lo, [qdst(bi, 0, 128, idx * 128)], ctxb)
                    elif idx == 4:
                        fm_finish(raw, 128, w, BLK64, 64, 17, ropeA, lo, [kdst(bi, 0, 128, 0)], ctxb)
                    elif idx == 5:
                        raw5 = raw
                    elif idx == 6:
                        ss = PB.next()
                        for k, rp in enumerate((raw5, raw)):
                            sq = sq_p.next()
                            S.op("act", lambda e, sq=sq, rp=rp, w=w: e.activation(sq[:, 0:w], rp[:, 0:w], AF.Square), reads=[rp], writes=[sq])
                            mm(ss, ss[:, 0:w], cmat, cmat[:, ONES, :], sq, sq[:, 0:w], k == 0, k == 1)
                        rstd = f32_p.next()
                        S.op("act", lambda e, rstd=rstd, ss=ss, w=w: e.activation(rstd[:, 0:w], ss[:, 0:w], AF.Sqrt, bias=EPS * 256, scale=1.0), reads=[ss], writes=[rstd])
                        S.op("dve", lambda e, rstd=rstd, ss=ss, w=w: e.reciprocal(rstd[:, 0:w], rstd[:, 0:w]), reads=[rstd], writes=[rstd])
                        for k, rp in enumerate((raw5, raw)):
                            S.op("dve", lambda e, k=k, rp=rp, rstd=rstd, w=w, ho=ho: e.scalar_tensor_tensor(
                                cqn[:, k, ho:ho + w], rp[:, 0:w], gg[:, 18 + k:19 + k], rstd[:, 0:w], ALU.mult, ALU.mult),
                                reads=[rp, gg, rstd], writes=[cqn])
                    elif idx == 7:
                        sq = sq_p.next()
                        S.op("act", lambda e, sq=sq, raw=raw, w=w: e.activation(sq[:, 0:w], raw[:, 0:w], AF.Square), reads=[raw], writes=[sq])
                        ss = PB.next()
                        mm(ss, ss[:, 0:w], cmat, cmat[:, ONES, :], sq, sq[:, 0:w], True, True)
                        rstd = f32_p.next()
                        S.op("act", lambda e, rstd=rstd, ss=ss, w=w: e.activation(rstd[:, 0:w], ss[:, 0:w], AF.Sqrt, bias=EPS * 128, scale=1.0), reads=[ss], writes=[rstd])
                        S.op("dve", lambda e, rstd=rstd, ss=ss, w=w: e.reciprocal(rstd[:, 0:w], rstd[:, 0:w]), reads=[rstd], writes=[rstd])
                        S.op("dve", lambda e, raw=raw, rstd=rstd, w=w, ho=ho: e.scalar_tensor_tensor(
                            ckvn[:, ho:ho + w], raw[:, 0:w], gg[:, 20:21], rstd[:, 0:w], ALU.mult, ALU.mult),
                            reads=[raw, gg, rstd], writes=[ckvn])
                    elif idx < 12:
                        fm_finish(raw, 128, w, BLK32, 32, 25, ropeB, lo, [qdst(bi, 0, 128, 1280 + (idx - 8) * 128)], ctxb)
                    elif idx < 16:
                        fm_finish(raw, 128, w, BLK32, 32, 26, ropeB, lo, [kdst(bi, 0, 128, 672 + (idx - 12) * 128)], ctxb)
                    else:
                        fm_finish(raw, 32, w, BLK32, 32, 24, ropeB, lo, [kdst(bi, 0, 32, 640)], ctxb)
        wv1 = wchunk(w_l, FMW, 512)
        wv2 = wchunk(w_l, FMW + 512, 128)
        vreg = kv_loc[KT_ROWS:KV_ROWS, :].rearrange("r c -> (r c)").rearrange("(t v) -> t v", v=V_COLS)

        def vdst(bi, s_, c0, ncol):
            t0, w = TBS[bi]
            if bi == 0:
                return (vc_b, vc_d[s_ * 128:(s_ + 1) * 128, c0:c0 + ncol])
            tl = t0 - NCTX + s_ * 128
            return (kvloc_b, vreg[tl:tl + 128, c0:c0 + ncol])

        def tm_chunk(bi, s_, lhs_buf, lhs_aps, rhs_buf, rhs_aps, ncol, vcol0):
            ps = PA.next()
            nk = len(lhs_aps)
            for k in range(nk):
                mm(ps, ps[:, 0:ncol], lhs_buf, lhs_aps[k], rhs_buf, rhs_aps[k], k == 0, k == nk - 1)
            ob = st_p.next()
            S.op("act", lambda e: e.copy(ob[:, 0:ncol], ps[:, 0:ncol]), reads=[ps], writes=[ob])
            dbuf, dap = vdst(bi, s_, vcol0, ncol)
            dma_out(dbuf, dap, ob, ob[:, 0:ncol])

        for bi in blocks:
            t0, w = TBS[bi]
            ho = t0 - gt0
            for s_ in range(w // 128):
                hs = [hT[:, k, ho + s_ * 128:ho + (s_ + 1) * 128] for k in range(8)]
                tm_chunk(bi, s_, hT, hs, wv1, [wv1[:, k, 0:128] for k in range(8)], 128, 0)
                tm_chunk(bi, s_, hT, hs, wv1, [wv1[:, k, 128:512] for k in range(8)], 384, 640)
                tm_chunk(bi, s_, hT, hs, wv2, [wv2[:, k, 0:128] for k in range(8)], 128, 640 + 384)
        for bi in blocks:
            t0, w = TBS[bi]
            ho = t0 - gt0
            lo = t0 - NCTX - lat0
            ctxb = (bi == 0)
            for i in range(4):
                raw = PA.next()
                for k in range(2):
                    mm(raw, raw[:, 0:w], wuq, wuq[:, k, i * 128:(i + 1) * 128], cqn, cqn[:, k, ho:ho + w], k == 0, k == 1)
                fm_finish(raw, 128, w, BLK64, 64, 21, None, lo,
                          [qdst(bi, 0, 64, 512 + (2 * i) * 96), qdst(bi, 64, 64, 512 + (2 * i + 1) * 96)], ctxb)
            for i in range(2):
                raw = PA.next()
                for k in range(2):
                    mm(raw, raw[:, 0:w], wuq, wuq[:, k, 512 + i * 128:512 + (i + 1) * 128], cqn, cqn[:, k, ho:ho + w], k == 0, k == 1)
                fm_finish(raw, 128, w, BLK32, 32, 22, ropeB, lo,
                          [qdst(bi, 32 * j, 32, 512 + (4 * i + j) * 96 + 64) for j in range(4)], ctxb)
            for i in range(4):
                raw = PA.next()
                mm(raw, raw[:, 0:w], wukv, wukv[:, i * 128:(i + 1) * 128], ckvn, ckvn[:, ho:ho + w], True, True)
                fm_finish(raw, 128, w, BLK64, 64, 23, None, lo, [kdst(bi, 0, 128, 128 + i * 128)], ctxb)
            for s_ in range(w // 128):
                tm_chunk(bi, s_, ckvn, [ckvn[:, ho + s_ * 128:ho + (s_ + 1) * 128]], wukv, [wukv[:, 512:1024]], 512, 128)
        if dbg and dbg.get("dumpP") and g == 0:
            S.barrier()
            dsm = S.new_sem(16)
            for nm, bf_, shp in (("d_cqn", cqn, [128, 2, 1280]), ("d_ckvn", ckvn, [128, 1280]), ("d_wuq", wuq, [128, 2, 768]),
                                 ("d_wukv", wukv, [128, 1024]), ("d_hT", hT, [128, 8, 1280])):
                dd = nc.dram_tensor(nm, shp, BF16, kind="ExternalOutput").ap()
                idx_ = tuple(slice(None) for _ in shp)
                S.dma("sp", dd[idx_], bf_[idx_], dsm, reads=[bf_], writes=[])
            print("sbuf remaining", nc.sbuf_bytes_remaining)

    def head_table():
        hs = []
        for h in range(8):
            hs.append(dict(kind="A", krows=[((h // 4) * 64, 64)], vcol=(h // 4) * 64, qrows=[(h * 64, 64)],
                           scale=64 ** -0.5, orow=h * 64, maps=[(0, 64)]))
        for h in range(8):
            hs.append(dict(kind="B", krows=[(128 + h * 64, 64), (640, 32)], vcol=128 + h * 64,
                           qrows=[(512 + h * 96, 96)], scale=96 ** -0.5, orow=512 + h * 64, maps=[(0, 96)]))
        for h in range(8):
            hs.append(dict(kind="C", krows=[(672 + 2 * h * 32, 64)], vcol=640 + h * 64,
                           qrows=[(1280 + 2 * h * 32, 64)], scale=32 ** -0.5, orow=1024 + h * 64, maps=[(0, 32), (32, 32)]))
        return hs

    def emit_attention(li, last, heads=None):
        phase_start()
        ktb = [Buf(RING[0:96, j * 8192:(j + 1) * 8192], psem()) for j in range(2)]
        kcb = [carve([96, NCTX], BF16, dma=True) for _ in range(2)]
        vtb = [carve([128, NKT, 65], BF16, dma=True) for _ in range(2)]
        qtb = [carve([96, NT], BF16, dma=True) for _ in range(2)]
        pt_p = Pool([carve([128, 512], BF16) for _ in range(4)])
        osb_p = Pool([carve([128, 512], F32) for _ in range(2)])
        rl_p = Pool([carve([128, 512], F32) for _ in range(2)])
        o_p = Pool([carve([64, 512], F32) for _ in range(4)])
        on_p = Pool([carve([64, 512], BF16, dma=True) for _ in range(3)])
        gg = gains[li]
        nlm = neglam[li]
        hs = head_table()
        if heads is not None:
            hs = [hs[i] for i in heads]
        for vb in vtb:
            S.op("pool", lambda e, vb=vb: e.memset(vb[:, :, 64:65], 1.0), reads=[], writes=[vb])
        vreg_all = [kv_all[r * KV_ROWS + KT_ROWS:(r + 1) * KV_ROWS, :].rearrange("r c -> (r c)").rearrange("(t v) -> t v", v=V_COLS)
                    for r in range(4)]
        for hi, hd in enumerate(hs):
            kb = ktb[hi % 2]; kc = kcb[hi % 2]; vb = vtb[hi % 2]; qb = qtb[hi % 2]
            r_off = 0
            for (kr0, nr) in hd["krows"]:
                S.dma("sp", kc[r_off:r_off + nr, :], ktc_d[kr0:kr0 + nr, :], kc.dsem, reads=[ktc_b], writes=[kc])
                for r in range(4):
                    S.dma("sp", kb[r_off:r_off + nr, r * NLAT:(r + 1) * NLAT],
                          kv_all[r * KV_ROWS + kr0:r * KV_ROWS + kr0 + nr, :], kb.dsem, reads=[kvall_b], writes=[kb])
                r_off += nr
            vc0 = hd["vcol"]
            S.dma("sp", vb[:, 0:2, 0:64], vc_d[:, vc0:vc0 + 64].rearrange("(k p) c -> p k c", p=128), vb.dsem,
                  reads=[vc_b], writes=[vb])
            for r in range(4):
                S.dma("sp", vb[:, 2 + 16 * r:2 + 16 * (r + 1), 0:64],
                      vreg_all[r][:, vc0:vc0 + 64].rearrange("(k p) c -> p k c", p=128), vb.dsem,
                      reads=[kvall_b], writes=[vb])
            r_off = 0
            for (qr0, nr) in hd["qrows"]:
                S.dma("sp", qb[r_off:r_off + nr, :], qt_d[qr0:qr0 + nr, :], qb.dsem, reads=[qt_b], writes=[qb])
                r_off += nr
            orow = hd["orow"]
            for bi, (t0, w) in enumerate(TBS):
                if bi == 0 and last:
                    continue
                nkt = 2 if bi == 0 else NKT
                onorm = []
                for (mr0, dk) in hd["maps"]:
                    ops = PB.next()
                    spsl = {}

                    def issue_S(kt):
                        sps_ = PA.next()
                        if kt < 2:
                            mm(sps_, sps_[:, 0:w], kc, kc[mr0:mr0 + dk, kt * 128:(kt + 1) * 128], qb, qb[mr0:mr0 + dk, t0:t0 + w], True, True)
                        else:
                            mm(sps_, sps_[:, 0:w], kb, kb[mr0:mr0 + dk, (kt - 2) * 128:(kt - 1) * 128], qb, qb[mr0:mr0 + dk, t0:t0 + w], True, True)
                        spsl[kt] = sps_

                    LA = 2
                    for kt in range(min(LA, nkt)):
                        issue_S(kt)
                    for kt in range(nkt):
                        if kt + LA < nkt:
                            issue_S(kt + LA)
                        sps = spsl.pop(kt)
                        pt = pt_p.next()
                        S.op("act", lambda e, pt=pt, sps=sps, w=w, sc=hd["scale"]: e.activation(pt[:, 0:w], sps[:, 0:w], AF.Exp, scale=sc),
                             reads=[sps], writes=[pt])
                        mm(ops, ops[0:65, 0:w], vb, vb[:, kt, 0:65], pt, pt[:, 0:w], kt == 0, kt == nkt - 1)
                    rl = rl_p.next()
                    S.op("dve", lambda e, rl=rl, ops=ops, w=w: e.reciprocal(rl[64:65, 0:w], ops[64:65, 0:w]), reads=[ops], writes=[rl])
                    osb = osb_p.next()
                    S.op("act", lambda e, osb=osb, ops=ops, w=w: e.copy(osb[0:64, 0:w], ops[0:64, 0:w]), reads=[ops], writes=[osb])
                    bc = PC.next()
                    mm(bc, bc[0:64, 0:w], c32, c32[64:65, 0, 0:64], rl, rl[64:65, 0:w], True, True)
                    if hd["kind"] == "C":
                        o_ = o_p.next()
                    else:
                        o_ = on_p.next()
                    S.op("dve", lambda e, o_=o_, osb=osb, bc=bc, w=w: e.tensor_tensor(o_[:, 0:w], osb[0:64, 0:w], bc[0:64, 0:w], ALU.mult),
                         reads=[osb, bc], writes=[o_])
                    onorm.append(o_)
                if hd["kind"] == "C":
                    o1b, o2 = onorm
                    od = o_p.next()
                    S.op("dve", lambda e, od=od, o2=o2, o1b=o1b, w=w: e.scalar_tensor_tensor(
                        od[:, 0:w], o2[:, 0:w], nlm[0:64, 0:1], o1b[:, 0:w], ALU.mult, ALU.add),
                        reads=[o2, o1b, nlm], writes=[od])
                    sq = sq_p.next()
                    S.op("act", lambda e, sq=sq, od=od, w=w: e.activation(sq[0:64, 0:w], od[:, 0:w], AF.Square), reads=[od], writes=[sq])
                    ss = PC.next()
                    mm(ss, ss[0:64, 0:w], cmat, cmat[0:64, BLK64, 0:64], sq, sq[0:64, 0:w], True, True)
                    rstd = f32_p.next()
                    S.op("act", lambda e, rstd=rstd, ss=ss, w=w: e.activation(rstd[0:64, 0:w], ss[0:64, 0:w], AF.Sqrt, bias=EPS * 64, scale=1.0), reads=[ss], writes=[rstd])
                    S.op("dve", lambda e, rstd=rstd, ss=ss, w=w: e.reciprocal(rstd[0:64, 0:w], rstd[0:64, 0:w]), reads=[rstd], writes=[rstd])
                    on = on_p.next()
                    S.op("dve", lambda e, on=on, od=od, rstd=rstd, w=w: e.scalar_tensor_tensor(
                        on[:, 0:w], od[:, 0:w], gg[0:64, 27:28], rstd[0:64, 0:w], ALU.mult, ALU.mult),
                        reads=[od, gg, rstd], writes=[on])
                else:
                    on = onorm[0]
                dma_out(ot_b, ot_d[orow:orow + 64, t0:t0 + w], on, on[:, 0:w])

    def emit_merge(li, g, last):
        phase_start()
        hT = carve([128, 8, 1280], BF16)
        otg = Pool([carve([128, 4, 1280], BF16, dma=True) for _ in range(2)])
        yT = carve([128, 8, 1280], BF16)
        wo_p = Pool([carve([128, 4, 512], BF16, dma=True) for _ in range(2)])
        gt0, gt1 = group_range(g)
        blocks = [b for b in GROUPS[g] if not (last and b == 0)]
        ab = AB[li]
        for bi in blocks:
            emit_norm_mod(li, bi, 0, hT, TBS[bi][0] - gt0)
        wos = [woa_d[li], wob_d[li], woc_d[li]]
        for m in range(3):
            og = otg.next()
            dma_in(og, og[:, :, 0:gt1 - gt0],
                   ot_d[m * 512:(m + 1) * 512, gt0:gt1].rearrange("(c p) t -> p c t", p=128), extra_reads=[ot_b])
            for jg in range(2):
                gw = wchunk(winp_d[li], FMW + TMW + m * 1024 + jg * 512, 512)
                ow = wo_p.next()
                dma_in(ow, ow[:, :, :], wos[m][:, jg * 512:(jg + 1) * 512].rearrange("(c p) n -> p c n", p=128), q="pool")
                for bi in blocks:
                    t0, w = TBS[bi]
                    ho = t0 - gt0
                    for j4 in range(4):
                        j = jg * 4 + j4
                        pp = PA.next()
                        for kc_ in range(4):
                            mm(pp, pp[:, 0:w], ow, ow[:, kc_, j4 * 128:(j4 + 1) * 128], og, og[:, kc_, ho:ho + w], kc_ == 0, kc_ == 3)
                        gp = PB.next()
                        for dc in range(8):
                            mm(gp, gp[:, 0:w], gw, gw[:, dc, j4 * 128:(j4 + 1) * 128], hT, hT[:, dc, ho:ho + w], dc == 0, dc == 7)
                        sg = sig_p.next()
                        S.op("act", lambda e, sg=sg, gp=gp, w=w: e.activation(sg[:, 0:w], gp[:, 0:w], AF.Sigmoid), reads=[gp], writes=[sg])
                        if m == 0:
                            S.op("dve", lambda e, sg=sg, pp=pp, j=j, ho=ho, w=w: e.tensor_tensor(yT[:, j, ho:ho + w], pp[:, 0:w], sg[:, 0:w], ALU.mult),
                                 reads=[pp, sg], writes=[yT])
                        else:
                            tmp = f32_p.next()
                            S.op("dve", lambda e, tmp=tmp, sg=sg, pp=pp, w=w: e.tensor_tensor(tmp[:, 0:w], pp[:, 0:w], sg[:, 0:w], ALU.mult),
                                 reads=[pp, sg], writes=[tmp])
                            S.op("pool", lambda e, tmp=tmp, j=j, ho=ho, w=w: e.tensor_tensor(yT[:, j, ho:ho + w], yT[:, j, ho:ho + w], tmp[:, 0:w], ALU.add),
                                 reads=[tmp, yT], writes=[yT])
        for jg in range(2):
            ww = wchunk(wout_d[li], jg * 512, 512)
            for bi in blocks:
                t0, w = TBS[bi]
                ho = t0 - gt0
                cond = 1 if bi == 0 else 0
                for j4 in range(4):
                    j = jg * 4 + j4
                    zp = PA.next()
                    for dc in range(8):
                        mm(zp, zp[:, 0:w], ww, ww[:, dc, j4 * 128:(j4 + 1) * 128], yT, yT[:, dc, ho:ho + w], dc == 0, dc == 7)
                    x = xT[bi]
                    S.op("dve", lambda e, zp=zp, x=x, j=j, w=w, cond=cond: e.scalar_tensor_tensor(
                        x[:, j, :], zp[:, 0:w], ab[:, 2, j, cond:cond + 1], x[:, j, :], ALU.mult, ALU.add),
                        reads=[zp, ab, x], writes=[x])

    def emit_moe(li, last, n_exp=32):
        phase_start()
        h2 = [carve([128, 8, w], BF16) for (_, w) in TBS]
        o_act = uoff[0]
        act_all = carve([128, 4, NT], BF16)
        actT = [Buf(act_all[:, :, t0:t0 + w]) for (t0, w) in TBS]
        h2f = Buf(UF[:, o_act // 2:o_act // 2 + 4096].rearrange("p (c t) -> p c t", t=512))
        GT = carve([32, NT], BF16)
        GTm = Pool([carve([32, 512], BF16) for _ in range(2)])
        rw = carve([128, 8, 32], F32, dma=True)
        rb = carve([128, 32], F32, dma=True)
        bgu = carve([128, 32, 16], F32, dma=True)
        bgu1 = carve([128, 32, 8], F32)
        bdn = carve([32, 1024], BF16, dma=True)
        rt = carve([128, 4, 48], F32)
        gbc_p = Pool([carve([128, 512], BF16) for _ in range(2)])
        blocks = [b for b in range(5) if not (last and b == 0)]
        ab = AB[li]
        dma_in(rw, rw[:, :, :], rw_d[li])
        dma_in(rb, rb[:, :], rb_d[li])
        dma_in(bgu, bgu[:, :, :], bgu_d[li])
        dma_in(bdn, bdn[:, :], bdn_d[li], q="pool")
        S.op("dve", lambda e: e.tensor_scalar(bgu1[:, :, :], bgu[:, :, 8:16], 1.0, None, ALU.add), reads=[bgu], writes=[bgu1])
        for bi in blocks:
            t0, w = TBS[bi]
            emit_norm_mod(li, bi, 1, h2[bi], 0, f32_dst=h2f)
            for s_ in range(w // 128):
                lp = PC.next()
                for dc in range(8):
                    mm(lp, lp[:, 0:32], h2f, h2f[:, dc, s_ * 128:(s_ + 1) * 128], rw, rw[:, dc, :], dc == 0, dc == 7)
                r = rt
                S.op("dve", lambda e, lp=lp: e.tensor_tensor(r[:, 0, 0:32], lp[:, 0:32], rb[:, :], ALU.add), reads=[lp, rb], writes=[r])
                S.op("dve", lambda e: e.max(r[:, 1, 0:8], r[:, 0, 0:32]), reads=[r], writes=[r])
                S.op("dve", lambda e: e.tensor_scalar(r[:, 1, 8:9], r[:, 1, 0:1], -1.0, None, ALU.mult), reads=[r], writes=[r])
                S.op("dve", lambda e: e.tensor_scalar(r[:, 2, 0:32], r[:, 0, 0:32], r[:, 1, 3:4], None, ALU.is_ge), reads=[r], writes=[r])
                S.op("act", lambda e: e.activation(r[:, 3, 0:32], r[:, 0, 0:32], AF.Exp, bias=r[:, 1, 8:9]), reads=[r], writes=[r])
                S.op("dve", lambda e: e.tensor_tensor(r[:, 3, 0:32], r[:, 3, 0:32], r[:, 2, 0:32], ALU.mult), reads=[r], writes=[r])
                S.op("dve", lambda e: e.reduce_sum(r[:, 1, 9:10], r[:, 3, 0:32], AX.X), reads=[r], writes=[r])
                S.op("dve", lambda e: e.reciprocal(r[:, 1, 10:11], r[:, 1, 9:10]), reads=[r], writes=[r])
                S.op("dve", lambda e: e.tensor_scalar(r[:, 3, 0:32], r[:, 3, 0:32], r[:, 1, 10:11], None, ALU.mult), reads=[r], writes=[r])
                tp = PC.next()
                S.op("pe", lambda e, tp=tp: e.transpose(tp[0:32, 0:128], r[:, 3, 0:32], c32[:, 1, :]), reads=[r, c32], writes=[tp])
                S.op("act", lambda e, tp=tp, t0=t0, s_=s_: e.copy(GT[:, t0 + s_ * 128:t0 + (s_ + 1) * 128], tp[0:32, 0:128]), reads=[tp], writes=[GT])
        S.barrier()
        if dbg and dbg.get("dumpM"):
            dsm = S.new_sem(16)
            dd = nc.dram_tensor("d_GT", [32, NT], BF16, kind="ExternalOutput").ap()
            S.dma("sp", dd[:, :], GT[:, :], dsm, reads=[GT], writes=[])
            for bi_ in range(5):
                dd = nc.dram_tensor("d_h2_%d" % bi_, [128, 8, TBS[bi_][1]], BF16, kind="ExternalOutput").ap()
                S.dma("sp", dd[:, :, :], h2[bi_][:, :, :], dsm, reads=[h2[bi_]], writes=[])
            S.barrier()
        for bi in blocks:
            t0, w = TBS[bi]
            cond = 1 if bi == 0 else 0
            x = xT[bi]
            for j in range(8):
                bp = PC.next()
                mm(bp, bp[:, 0:w], bdn, bdn[:, j * 128:(j + 1) * 128], GT, GT[:, t0:t0 + w], True, True)
                S.op("dve", lambda e, bp=bp, x=x, j=j, w=w, cond=cond: e.scalar_tensor_tensor(
                    x[:, j, :], bp[:, 0:w], ab[:, 5, j, cond:cond + 1], x[:, j, :], ALU.mult, ALU.add),
                    reads=[bp, ab, x], writes=[x])
        pgP = Pool(banks[0:3])
        plP = Pool([banks[3], banks[4], banks[7]])
        for ex in range(n_exp):
            wg = wgu_d[li, ex]
            wd = wdn_d[li, ex]
            for half in range(2):
                gl = wchunk(wg, half * 512, 512)
                ln = wchunk(wg, 1024 + half * 512, 512)
                pending = None
                for bi in blocks:
                    t0, w = TBS[bi]
                    gm_ = GTm.next()
                    S.op("dve", lambda e, gm_=gm_, t0=t0, w=w, ex=ex: e.tensor_scalar(
                        gm_[:, 0:w], GT[:, t0:t0 + w], c32[0:32, 1, ex:ex + 1], None, ALU.mult), reads=[GT, c32], writes=[gm_])
                    gps = PC.next()
                    mm(gps, gps[:, 0:w], cmat, cmat[0:32, ONES, :], gm_, gm_[:, 0:w], True, True)
                    gb = gbc_p.next()
                    S.op("act", lambda e, gb=gb, gps=gps, w=w: e.copy(gb[:, 0:w], gps[:, 0:w]), reads=[gps], writes=[gb])
                    for f4 in range(4):
                        fc = half * 4 + f4
                        pg = pgP.next()
                        pl = plP.next()
                        for dc in range(8):
                            mm(pg, pg[:, 0:w], gl, gl[:, dc, f4 * 128:(f4 + 1) * 128], h2[bi], h2[bi][:, dc, :], dc == 0, dc == 7)
                        for dc in range(8):
                            mm(pl, pl[:, 0:w], ln, ln[:, dc, f4 * 128:(f4 + 1) * 128], h2[bi], h2[bi][:, dc, :], dc == 0, dc == 7)
                        gv = f32_p.next()
                        S.op("dve", lambda e, gv=gv, pg=pg, fc=fc, w=w, ex=ex: e.tensor_scalar(
                            gv[:, 0:w], pg[:, 0:w], bgu[:, ex, fc:fc + 1], 7.0, ALU.add, ALU.min), reads=[pg, bgu], writes=[gv])
                        sg = sig_p.next()
                        S.op("act", lambda e, sg=sg, gv=gv, w=w: e.activation(sg[:, 0:w], gv[:, 0:w], AF.Sigmoid, scale=1.702),
                             reads=[gv], writes=[sg])
                        lv = f32_p.next()
                        S.op("dve", lambda e, lv=lv, pl=pl, fc=fc, w=w, ex=ex: e.tensor_scalar(
                            lv[:, 0:w], pl[:, 0:w], bgu1[:, ex, fc:fc + 1], -6.0, ALU.add, ALU.max), reads=[pl, bgu1], writes=[lv])
                        S.op("pool", lambda e, gv=gv, sg=sg, w=w: e.tensor_tensor(gv[:, 0:w], gv[:, 0:w], sg[:, 0:w], ALU.mult),
                             reads=[gv, sg], writes=[gv])
                        S.op("pool", lambda e, gv=gv, gb=gb, w=w: e.tensor_tensor(gv[:, 0:w], gv[:, 0:w], gb[:, 0:w], ALU.mult),
                             reads=[gv, gb], writes=[gv])
                        at = actT[bi]
                        if pending is not None:
                            pending()

                        def pending(at=at, gv=gv, lv=lv, f4=f4, w=w):
                            S.op("dve", lambda e: e.scalar_tensor_tensor(
                                at[:, f4, :], lv[:, 0:w], 8.0, gv[:, 0:w], ALU.min, ALU.mult),
                                reads=[gv, lv], writes=[at])
                if pending is not None:
                    pending()
                    pending = None
                dwb = ring.next()
                dwv = RING_view(dwb)
                S.dma("pool", dwv, wd[half * 512:(half + 1) * 512, :].rearrange("(c p) n -> p c n", p=128), dwb.dsem, reads=[], writes=[dwb])
                for bi in blocks:
                    t0, w = TBS[bi]
                    cond = 1 if bi == 0 else 0
                    x = xT[bi]
                    at = actT[bi]
                    for j in range(8):
                        yp = pgP.next()
                        for f4 in range(4):
                            mm(yp, yp[:, 0:w], dwb, dwv[:, f4, j * 128:(j + 1) * 128], at, at[:, f4, :], f4 == 0, f4 == 3)
                        S.op("dve", lambda e, yp=yp, x=x, j=j, w=w, cond=cond: e.scalar_tensor_tensor(
                            x[:, j, :], yp[:, 0:w], ab[:, 5, j, cond:cond + 1], x[:, j, :], ALU.mult, ALU.add),
                            reads=[yp, ab, x], writes=[x])

    def RING_view(b):
        return b.ap.rearrange("p c n -> p (c n)").rearrange("p (c n) -> p c n", n=1024)

    def emit_exchange():
        if mode != "fused":
            return
        E = S.E["pool"]
        waits = S._waits(E, [kvloc_b], [kvall_b])
        ccs = S.new_sem(16)
        ccs.n += 1
        E.prog.append((waits, lambda e: e.collective_compute(
            "AllGather", ALU.bypass, [[0, 1, 2, 3], [4, 5, 6, 7]],
            [kv_loc[:, :]], [kv_all[:, :]]), None, (ccs, 1)))
        kvloc_b.r[ccs] = 1
        kvall_b.w = (ccs, 1)
        kvall_b.r = {}

    stop = dbg.get("stop") if dbg else None
    for li in range(nl):
        emit_modulation(li)
    for li in range(nl):
        last = (layers[li] == L - 1)
        emit_phase_P(li, 0)
        emit_phase_P(li, 1)
        if mode == "A" or stop == "P":
            break
        emit_exchange()
        emit_attention(li, last, heads=dbg.get("heads") if dbg else None)
        if stop == "attn":
            break
        emit_merge(li, 0, last)
        emit_merge(li, 1, last)
        if stop == "merge":
            break
        emit_moe(li, last, n_exp=dbg.get("n_exp", 32) if dbg else 32)
    S.barrier()
    if mode == "A":
        S.finish("sp", [kvloc_b])
    else:
        outb = Buf(out_d)
        osem = S.new_sem(16)
        for bi, (t0, w) in enumerate(TBS):
            S.dma("sp", out_d[:, :, t0:t0 + w], xT[bi][:, :, :], osem, reads=[xT[bi]], writes=[outb])
        S.finish("sp", [outb])
    S.emit()
    return nc


def _rope_tables(rot_dim, n_lat):
    n_rows = n_lat // 64
    row = np.repeat(np.arange(n_rows, dtype=np.float32), 64)
    col = np.tile(np.arange(64, dtype=np.float32), n_rows)
    axis_pairs = rot_dim // 4
    inv = (10000.0 ** (-np.arange(axis_pairs, dtype=np.float32) / axis_pairs)).astype(np.float32)
    ang = np.concatenate([row[:, None] * inv, col[:, None] * inv], axis=-1).astype(np.float32)
    return np.cos(ang).astype(np.float32), np.sin(ang).astype(np.float32)


def _const_mats():
    m = np.zeros((128, 6, 128), np.float32)
    m[:, 0, :] = 1.0
    for i in range(128):
        for j in range(128):
            if i // 64 == j // 64:
                m[i, 1, j] = 1.0
            if i // 32 == j // 32:
                m[i, 2, j] = 1.0
    for (slot, blk) in ((3, 64), (4, 32)):
        hh = blk // 2
        for mm_ in range(128):
            i = mm_ % blk
            base = mm_ - i
            if i < hh:
                m[base + i + hh, slot, mm_] = -1.0
            else:
                m[base + i - hh, slot, mm_] = 1.0
    m[:, 5, :] = np.eye(128, dtype=np.float32)
    return m


def _feat_major(v):
    return np.ascontiguousarray(v.reshape(8, 128).T)


def _prep_shared(inp, layers):
    f = np.float32
    out = {}
    ls = list(layers)
    sel = np.zeros((32, 32, 128), f)
    for e in range(32):
        sel[e, e, :] = 1.0
    out["sel"] = sel
    out["cmat"] = _const_mats()
    gm = np.zeros((len(ls), 128, NG), f)
    gains = np.zeros((len(ls), 128, NG), f)
    for i, l in enumerate(ls):
        lam_init = 0.8 - 0.6 * np.exp(-0.3 * l)
        gm[i, :, 0:16] = 32.0
        gm[i, :, 16] = 8.0; gm[i, :, 17] = 8.0
        gm[i, :, 18:20] = 16.0
        gm[i, :, 20] = np.sqrt(128.0)
        gm[i, :, 21] = 8.0; gm[i, :, 22] = np.sqrt(32.0); gm[i, :, 23] = 8.0; gm[i, :, 24] = np.sqrt(32.0)
        gm[i, :, 25] = np.sqrt(32.0); gm[i, :, 26] = np.sqrt(32.0)
        gm[i, :, 27] = 8.0 * (1.0 - lam_init)
        gains[i, :, 0:8] = _feat_major(inp["norm_mix"][l])
        gains[i, :, 8:16] = _feat_major(inp["norm_ffn"][l])
        gains[i, :, 16] = np.tile(inp["a_q_norm"][l], 2)
        gains[i, :, 17] = np.tile(inp["a_k_norm"][l], 2)
        gains[i, :, 18:20] = inp["b_q_a_norm"][l].reshape(2, 128).T
        gains[i, :, 20] = inp["b_kv_a_norm"][l]
        gains[i, :, 21] = np.tile(inp["b_q_norm"][l][:64], 2)
        gains[i, :, 22] = np.tile(inp["b_q_norm"][l][64:], 4)
        gains[i, :, 23] = np.tile(inp["b_k_norm"][l][:64], 2)
        gains[i, :, 24] = np.tile(inp["b_k_norm"][l][64:], 4)
        gains[i, :, 25] = np.tile(inp["c_q_norm"][l], 4)
        gains[i, :, 26] = np.tile(inp["c_k_norm"][l], 4)
        gains[i, :, 27] = np.tile(inp["c_subln"][l], 2)
    out["gmul"] = gm
    out["gains"] = gains
    out["clam"] = np.ascontiguousarray(np.broadcast_to(inp["c_lambda"][ls].reshape(len(ls), 1, 128), (len(ls), 128, 128))).astype(f)
    li_ = np.array([0.8 - 0.6 * np.exp(-0.3 * l) for l in ls], f)
    out["laminit"] = np.ascontiguousarray(np.broadcast_to(li_.reshape(-1, 1, 1), (len(ls), 128, 1))).astype(f)
    out["bmod"] = np.ascontiguousarray(inp["b_mod"][ls].reshape(len(ls), 48, 128).transpose(0, 2, 1))
    out["rb"] = np.ascontiguousarray(np.broadcast_to(inp["router_b"][ls][:, None, :], (len(ls), 128, 32))).astype(f)
    out["bgu"] = np.ascontiguousarray(inp["exp_b_gu"][ls].reshape(len(ls), 32, 16, 128).transpose(0, 3, 1, 2))
    out["bdn"] = np.ascontiguousarray(inp["exp_b_down"][ls])
    out["rw"] = np.ascontiguousarray(inp["router_w"][ls].reshape(len(ls), 8, 128, 32).transpose(0, 2, 1, 3))
    out["wmod"] = np.ascontiguousarray(inp["w_mod"][ls])
    w_in = inp["w_in"][ls]
    sp = np.cumsum([0, 512, 128, 128, 256, 128, 32, 512, 512, 512, 3072])
    a_q, a_k, a_v, b_cq, b_ckv, b_kr, c_q, c_k, c_v, gates = [w_in[:, :, sp[i]:sp[i + 1]] for i in range(10)]
    pad = np.zeros((len(ls), 1024, 96), f)
    out["winp"] = np.ascontiguousarray(np.concatenate([a_q, a_k, b_cq, b_ckv, c_q, c_k, b_kr, pad, a_v, c_v, gates], axis=2))
    uq = inp["b_w_uq"][ls].reshape(len(ls), 256, 8, 96)
    out["wuq"] = np.ascontiguousarray(np.concatenate([uq[..., :64].reshape(len(ls), 256, 512), uq[..., 64:].reshape(len(ls), 256, 256)], axis=2))
    ukv = inp["b_w_ukv"][ls].reshape(len(ls), 128, 8, 128)
    out["wukv"] = np.ascontiguousarray(np.concatenate([ukv[..., :64].reshape(len(ls), 128, 512), ukv[..., 64:].reshape(len(ls), 128, 512)], axis=2))
    out["woa"] = np.ascontiguousarray(inp["w_o_a"][ls]); out["wob"] = np.ascontiguousarray(inp["w_o_b"][ls]); out["woc"] = np.ascontiguousarray(inp["w_o_c"][ls])
    out["wout"] = np.ascontiguousarray(inp["w_out"][ls])
    out["wgu"] = np.ascontiguousarray(inp["exp_w_gu"][ls])
    out["wdn"] = np.ascontiguousarray(inp["exp_w_down"][ls])
    return out


def _prep_core(inp, core, x_lat, x_ctx):
    b, r = core // 4, core % 4
    f = np.float32
    xt = np.concatenate([x_ctx[b], x_lat[b, r * NLAT:(r + 1) * NLAT]], axis=0)
    xT = np.ascontiguousarray(xt.reshape(NT, 8, 128).transpose(2, 1, 0)).astype(f)
    cond = np.stack([inp["c"][b], inp["c_ctx"]], axis=-1)
    condT = np.ascontiguousarray(cond.reshape(8, 128, 2).transpose(1, 0, 2)).astype(f)
    out = {"xT_in": xT, "condT": condT}
    for nm, rd, blk in (("A", 64, 64), ("B", 32, 32)):
        cs, sn = _rope_tables(rd, 8192)
        cs = cs[r * NLAT:(r + 1) * NLAT]; sn = sn[r * NLAT:(r + 1) * NLAT]
        idx = (np.arange(128) % blk) % (rd // 2)
        out["cos" + nm] = np.ascontiguousarray(cs[:, idx].T).astype(f)
        out["sin" + nm] = np.ascontiguousarray(sn[:, idx].T).astype(f)
    return out


def _unpack_x(res_list):
    x_lat = np.zeros((2, 8192, 1024), np.float32)
    x_ctx = np.zeros((2, 256, 1024), np.float32)
    for core, o in enumerate(res_list):
        b, r = core // 4, core % 4
        xt = np.asarray(o).transpose(2, 1, 0).reshape(NT, 1024)
        x_lat[b, r * NLAT:(r + 1) * NLAT] = xt[NCTX:]
        if r == 0:
            x_ctx[b] = xt[:NCTX]
    return x_lat, x_ctx


A_KEYS = ("xT_in", "condT", "cosA", "sinA", "cosB", "sinB", "cmat", "gmul", "gains", "clam", "laminit", "bmod",
          "wmod", "winp", "wuq", "wukv")


def _filter(m, mode):
    if mode == "A":
        return {k: v for k, v in m.items() if k in A_KEYS}
    return {k: v for k, v in m.items() if k != "sel"}


_CACHE = {}


def _get_prog(mode, last):
    key = (mode, bool(last))
    if key not in _CACHE:
        _CACHE[key] = build_program(mode, [L - 1 if last else 0])
    return _CACHE[key]


def kernel(**inp):
    inp = {k: np.asarray(v) for k, v in inp.items()}
    x_lat = np.ascontiguousarray(inp["x"], dtype=np.float32)
    x_ctx = np.ascontiguousarray(inp["ctx"], dtype=np.float32)
    for l in range(L):
        last = (l == L - 1)
        shared = _prep_shared(inp, [l])
        maps = []
        for core in range(8):
            m = dict(shared)
            m.update(_prep_core(inp, core, x_lat, x_ctx))
            maps.append(m)
        resA = run_bass_kernel_spmd(_get_prog("A", False), [_filter(m, "A") for m in maps], core_ids=list(range(8)))
        kv = [np.asarray(r["kv_loc"]) for r in resA.results]
        for core in range(8):
            b = core // 4
            maps[core]["kv_all"] = np.ascontiguousarray(np.concatenate(kv[4 * b:4 * b + 4], axis=0))
        resB = run_bass_kernel_spmd(_get_prog("B", last), [_filter(m, "B") for m in maps], core_ids=list(range(8)))
        x_lat, x_ctx_new = _unpack_x([r["out"] for r in resB.results])
        if not last:
            x_ctx = x_ctx_new
    return x_lat.astype(np.float32)
```

```python
import numpy as np
import concourse.bass as bass
import concourse.mybir as mybir
from concourse.bass_utils import run_bass_kernel_spmd

F32 = mybir.dt.float32
BF16 = mybir.dt.bfloat16
ALU = mybir.AluOpType
AF = mybir.ActivationFunctionType
AX = mybir.AxisListType

L = 4
D = 1024
NLAT = 2048
NCTX = 256
NT = NCTX + NLAT
TBS = [(0, 256), (256, 512), (768, 512), (1280, 512), (1792, 512)]
GROUPS = [[0, 1, 2], [3, 4]]
NKT = 66
NKEY = NKT * 128
EPS = 1e-6
KT_ROWS = 1184
V_COLS = 1152
KV_ROWS = KT_ROWS + V_COLS
QT_ROWS = 1792
OT_ROWS = 1536
NG = 28
FMW = 2176
TMW = 640
WINP = FMW + TMW + 3072


class Sem:
    def __init__(self, h, step):
        self.h = h
        self.step = step
        self.n = 0
        self.signal = None


class Eng:
    def __init__(self, name, sem):
        self.name = name
        self.sem = sem
        self.prog = []
        self.seen = {}
        self.sig = set()


class Buf:
    def __init__(self, ap, dsem=None):
        self.ap = ap
        self.w = None
        self.r = {}
        self.dsem = dsem

    def __getitem__(self, k):
        return self.ap[k]


class Sched:
    def __init__(self, nc):
        self.nc = nc
        self.sems_used = 0
        self.dsems = []
        self.E = {}
        for name in ("pe", "act", "dve", "pool", "sp"):
            self.E[name] = Eng(name, self.new_sem(1))

    def new_sem(self, step):
        self.sems_used += 1
        sm = Sem(self.nc.alloc_semaphore(name=f"s{self.sems_used}"), step)
        if step == 16:
            self.dsems.append(sm)
        return sm

    def barrier(self):
        for E in self.E.values():
            waits = []
            for E2 in self.E.values():
                if E2 is E or E2.sem.n == 0:
                    continue
                if E.seen.get(E2.sem, 0) < E2.sem.n:
                    E.seen[E2.sem] = E2.sem.n
                    waits.append((E2.sem, E2.sem.n))
                    E2.sig.add(E2.sem.n)
            for sm in self.dsems:
                if sm.n > 0 and E.seen.get(sm, 0) < sm.n:
                    E.seen[sm] = sm.n
                    waits.append((sm, sm.n))
            E.prog.append((waits, None, None, None))

    def _waits(self, E, reads, writes):
        deps = {}

        def add(t):
            if t is None:
                return
            s, n = t
            if s is E.sem and E.name in ("pe", "sp"):
                return
            if deps.get(s, 0) < n:
                deps[s] = n

        for b in reads:
            add(b.w)
        for b in writes:
            add(b.w)
            for s, n in b.r.items():
                add((s, n))
        waits = []
        for s, n in deps.items():
            if E.seen.get(s, 0) < n:
                E.seen[s] = n
                waits.append((s, n))
                if s.step == 1:
                    s.owner.sig.add(n)
        return waits

    def op(self, ename, fn, reads=(), writes=()):
        E = self.E[ename]
        waits = self._waits(E, reads, writes)
        E.sem.n += 1
        n = E.sem.n
        E.prog.append((waits, fn, n, None))
        for b in reads:
            b.r[E.sem] = n
        for b in writes:
            b.w = (E.sem, n)
            b.r = {}

    def dma(self, ename, out_ap, in_ap, sem, reads=(), writes=()):
        E = self.E[ename]
        waits = self._waits(E, reads, writes)
        sem.n += 1
        n = sem.n
        E.prog.append((waits, lambda e: e.dma_start(out=out_ap, in_=in_ap), None, (sem, n)))
        for b in reads:
            b.r[sem] = n
        for b in writes:
            b.w = (sem, n)
            b.r = {}

    def finish(self, ename, bufs):
        E = self.E[ename]
        waits = self._waits(E, bufs, [])
        E.prog.append((waits, None, None, None))

    def emit(self):
        for E in self.E.values():
            E.sem.owner = E
        for E in self.E.values():
            E.sigl = sorted(E.sig)
            E.rank = {n: i + 1 for i, n in enumerate(E.sigl)}

        def val(s, n):
            if s.step == 16:
                return 16 * n
            return s.owner.rank[n]

        def run(E, e):
            for waits, fn, n, dm in E.prog:
                for s, nn in waits:
                    e.wait_ge(s.h, val(s, nn))
                if fn is None:
                    continue
                ins = fn(e)
                if dm is not None:
                    ins.then_inc(dm[0].h, 16)
                elif n in E.sig:
                    ins.then_inc(E.sem.h, 1)

        with self.nc.Block() as block:
            @block.tensor
            def _(e):
                run(self.E["pe"], e)

            @block.scalar
            def _(e):
                run(self.E["act"], e)

            @block.vector
            def _(e):
                run(self.E["dve"], e)

            @block.gpsimd
            def _(e):
                run(self.E["pool"], e)

            @block.sync
            def _(e):
                run(self.E["sp"], e)


class Pool:
    def __init__(self, bufs):
        self.bufs = bufs
        self.i = 0

    def next(self):
        b = self.bufs[self.i % len(self.bufs)]
        self.i += 1
        return b


def build_program(mode, layers, n_ring=5, dbg=None):
    nc = bass.Bass("TRN2", target_bir_lowering=False)
    S = Sched(nc)
    for E in S.E.values():
        E.sem.owner = E
    nl = len(layers)

    def din(name, shape, dt=F32):
        return nc.dram_tensor(name, list(shape), dt, kind="ExternalInput").ap()

    def dout(name, shape, dt=F32):
        return nc.dram_tensor(name, list(shape), dt, kind="ExternalOutput").ap()

    def dint(name, shape, dt=BF16):
        if dbg:
            return nc.dram_tensor(name, list(shape), dt, kind="ExternalOutput").ap()
        return nc.dram_tensor(name, list(shape), dt, kind="Internal").ap()

    xT_in = din("xT_in", [128, 8, NT])
    condT = din("condT", [128, 8, 2])
    cosA_d = din("cosA", [128, NLAT]); sinA_d = din("sinA", [128, NLAT])
    cosB_d = din("cosB", [128, NLAT]); sinB_d = din("sinB", [128, NLAT])
    cmat_d = din("cmat", [128, 6, 128])
    gmul_d = din("gmul", [nl, 128, NG])
    gains_d = din("gains", [nl, 128, NG])
    clam_d = din("clam", [nl, 128, 128])
    laminit_d = din("laminit", [nl, 128, 1])
    bmod_d = din("bmod", [nl, 128, 48])
    wmod_d = din("wmod", [nl, 1024, 6144])
    winp_d = din("winp", [nl, 1024, WINP])
    wuq_d = din("wuq", [nl, 256, 768])
    wukv_d = din("wukv", [nl, 128, 1024])
    if mode != "A":
        rb_d = din("rb", [nl, 128, 32])
        bgu_d = din("bgu", [nl, 128, 32, 16])
        bdn_d = din("bdn", [nl, 32, 1024])
        rw_d = din("rw", [nl, 128, 8, 32])
        woa_d = din("woa", [nl, 512, 1024]); wob_d = din("wob", [nl, 512, 1024]); woc_d = din("woc", [nl, 512, 1024])
        wout_d = din("wout", [nl, 1024, 1024])
        wgu_d = din("wgu", [nl, 32, 1024, 2048])
        wdn_d = din("wdn", [nl, 32, 1024, 1024])

    if mode == "A":
        kv_loc = dout("kv_loc", [KV_ROWS, NLAT], BF16)
    else:
        kv_loc = dint("kv_loc", [KV_ROWS, NLAT])
    if mode == "B":
        kv_all = din("kv_all", [4 * KV_ROWS, NLAT], BF16)
    elif mode == "fused":
        kv_all = nc.dram_tensor("kv_all", [4 * KV_ROWS, NLAT], BF16, kind="Internal").ap()
    else:
        kv_all = None
    ktc_d = dint("ktc", [KT_ROWS, NCTX])
    vc_d = dint("vc", [NCTX, V_COLS])
    qt_d = dint("qt", [QT_ROWS, NT])
    ot_d = dint("ot", [OT_ROWS, NT])
    if mode != "A":
        out_d = dout("out", [128, 8, NT])

    kvloc_b = Buf(kv_loc); kvall_b = Buf(kv_all); ktc_b = Buf(ktc_d); vc_b = Buf(vc_d)
    qt_b = Buf(qt_d); ot_b = Buf(ot_d)

    cnt = [0]

    def sb(shape, dt, dma=False):
        cnt[0] += 1
        h = nc.alloc_sbuf_tensor(f"t{cnt[0]}", list(shape), dt)
        return Buf(h, S.new_sem(16) if dma else None)

    def sbpool(n, shape, dt, dma=False):
        return Pool([sb(shape, dt, dma) for _ in range(n)])

    USIZE = 36352
    phase_sems = []
    ps_idx = [0]

    def psem():
        if ps_idx[0] == len(phase_sems):
            phase_sems.append(S.new_sem(16))
        sm = phase_sems[ps_idx[0]]
        ps_idx[0] += 1
        return sm

    def phase_start():
        S.barrier()
        uoff[0] = 0
        ps_idx[0] = 0

    U = nc.alloc_sbuf_tensor("union", [128, USIZE], BF16)
    UF = U[:, :].bitcast(F32)
    uoff = [0]

    def carve(shape, dt, dma=False):
        per = 1
        for d_ in shape[1:]:
            per *= d_
        cols = per * (2 if dt == F32 else 1)
        cols_al = (cols + 15) // 16 * 16
        o = uoff[0]
        assert o + cols_al <= USIZE, ("union overflow", o, cols_al)
        uoff[0] = o + cols_al
        if dt == F32:
            ap = UF[0:shape[0], o // 2:(o + cols) // 2]
        else:
            ap = U[0:shape[0], o:o + cols]
        if len(shape) == 3:
            ap = ap.rearrange("p (a b) -> p a b", b=shape[2])
        elif len(shape) == 4:
            ap = ap.rearrange("p (a b c) -> p a b c", b=shape[2], c=shape[3])
        return Buf(ap, psem() if dma else None)

    banks = []
    for i in range(8):
        banks.append(Buf(nc.alloc_psum_tensor(f"ps{i}", [128, 512], F32)))
    PA = Pool(banks[0:3]); PB = Pool(banks[3:5]); PC = Pool(banks[5:7]); PD = Pool(banks[7:8])

    xT = [sb([128, 8, w], F32, dma=True) for (_, w) in TBS]
    cmat = sb([128, 6, 128], BF16, dma=True)
    c32 = sb([128, 2, 128], F32, dma=True)
    ONES, BLK64, BLK32, R64, R32, IDENT = range(6)
    silu_c = sb([128, 8, 2], BF16)
    cond32 = sb([128, 8, 2], F32, dma=True)
    modT = [sb([128, 48, 2], F32) for _ in layers]
    gains = [sb([128, NG], F32, dma=True) for _ in layers]
    gmul = sb([128, NG], F32, dma=True)
    bmod = sb([128, 48], F32, dma=True)
    AB = [sb([128, 6, 8, 2], F32) for _ in layers]
    neglam = [sb([128, 1], F32) for _ in layers]
    clam = sb([128, 128], F32, dma=True)
    laminit = sb([128, 1], F32, dma=True)
    lamtmp = sb([128, 68], F32)

    RING = nc.alloc_sbuf_tensor("ring", [128, n_ring * 4096], BF16)
    ring = Pool([Buf(RING[:, i * 4096:(i + 1) * 4096].rearrange("p (c n) -> p c n", n=512), S.new_sem(16)) for i in range(n_ring)])
    sq_p = sbpool(2, [128, 512], BF16)
    f32_p = sbpool(5, [128, 512], F32)
    sig_p = sbpool(2, [128, 512], F32)

    def dma_in(buf, dst_ap, src_ap, q="sp", extra_reads=()):
        S.dma(q, dst_ap, src_ap, buf.dsem, reads=list(extra_reads), writes=[buf])

    def dma_out(dst_buf, dst_ap, src_buf, src_ap, q="sp"):
        S.dma(q, dst_ap, src_ap, src_buf.dsem, reads=[src_buf], writes=[dst_buf])

    def mm(ps, ps_ap, lhsT_buf, lhsT_ap, rhs_buf, rhs_ap, start, stop):
        S.op("pe", lambda e: e.matmul(ps_ap, lhsT_ap, rhs_ap, start=start, stop=stop),
             reads=[lhsT_buf, rhs_buf], writes=[ps])

    for bi, (t0, w) in enumerate(TBS):
        dma_in(xT[bi], xT[bi][:, :, :], xT_in[:, :, t0:t0 + w])
    dma_in(cmat, cmat[:, :, :], cmat_d[:, :, :], q="pool")
    dma_in(c32, c32[:, 0, :], cmat_d[:, 0, :])
    dma_in(c32, c32[:, 1, :], cmat_d[:, 5, :])
    dma_in(cond32, cond32[:, :, :], condT[:, :, :])
    S.op("act", lambda e: e.activation(silu_c[:, :, :], cond32[:, :, :], AF.Silu), reads=[cond32], writes=[silu_c])

    def wchunk(w2d, c0, ncols, krows=1024):
        kc = krows // 128
        src = w2d[:, c0:c0 + ncols].rearrange("(c p) n -> p c n", p=128)
        b = ring.next()
        S.dma("pool", b[:, 0:kc, 0:ncols], src, b.dsem, reads=[], writes=[b])
        return b

    def emit_modulation(li):
        l_w = wmod_d[li]
        dma_in(gains[li], gains[li][:, :], gains_d[li])
        dma_in(gmul, gmul[:, :], gmul_d[li])
        dma_in(bmod, bmod[:, :], bmod_d[li])
        ps = PD.next()
        for cc in range(12):
            wb = wchunk(l_w, cc * 512, 512)
            for j4 in range(4):
                j = cc * 4 + j4
                for dc in range(8):
                    mm(ps, ps[:, 2 * j:2 * j + 2], wb, wb[:, dc, j4 * 128:(j4 + 1) * 128],
                       silu_c, silu_c[:, dc, :], dc == 0, dc == 7)
        m = modT[li]
        S.op("dve", lambda e: e.tensor_tensor(
            m[:, :, :], ps[:, 0:96].rearrange("p (j k) -> p j k", k=2),
            bmod[:, :].unsqueeze(2).to_broadcast([128, 48, 2]), ALU.add),
            reads=[ps, bmod], writes=[m])
        g = gains[li]
        S.op("dve", lambda e: e.tensor_tensor(g[:, :], g[:, :], gmul[:, :], ALU.mult),
             reads=[g, gmul], writes=[g])
        ab = AB[li]
        for k, (sc_j, sh_j, g_j, gcol) in enumerate([(8, 0, 16, 0), (32, 24, 40, 8)]):
            S.op("dve", lambda e, k=k, sc_j=sc_j, gcol=gcol: e.scalar_tensor_tensor(
                ab[:, 3 * k + 0, :, :], m[:, sc_j:sc_j + 8, :], 1.0,
                g[:, gcol:gcol + 8].unsqueeze(2).to_broadcast([128, 8, 2]), ALU.add, ALU.mult),
                reads=[m, g], writes=[ab])
            S.op("dve", lambda e, k=k, sh_j=sh_j: e.tensor_copy(ab[:, 3 * k + 1, :, :], m[:, sh_j:sh_j + 8, :]),
                 reads=[m], writes=[ab])
            S.op("dve", lambda e, k=k, g_j=g_j: e.tensor_copy(ab[:, 3 * k + 2, :, :], m[:, g_j:g_j + 8, :]),
                 reads=[m], writes=[ab])
        dma_in(clam, clam[:, :], clam_d[li])
        dma_in(laminit, laminit[:, :], laminit_d[li])
        lt = lamtmp
        S.op("dve", lambda e: e.tensor_tensor(lt[:, 0:32], clam[:, 0:32], clam[:, 32:64], ALU.mult), reads=[clam], writes=[lt])
        S.op("dve", lambda e: e.tensor_tensor(lt[:, 32:64], clam[:, 64:96], clam[:, 96:128], ALU.mult), reads=[clam], writes=[lt])
        S.op("dve", lambda e: e.reduce_sum(lt[:, 64:65], lt[:, 0:32], AX.X), reads=[lt], writes=[lt])
        S.op("dve", lambda e: e.reduce_sum(lt[:, 65:66], lt[:, 32:64], AX.X), reads=[lt], writes=[lt])
        S.op("act", lambda e: e.activation(lt[:, 66:68], lt[:, 64:66], AF.Exp), reads=[lt], writes=[lt])
        nlm = neglam[li]
        S.op("dve", lambda e: e.tensor_tensor(nlm[:, :], lt[:, 67:68], lt[:, 66:67], ALU.subtract), reads=[lt], writes=[nlm])
        S.op("dve", lambda e: e.tensor_tensor(nlm[:, :], nlm[:, :], laminit[:, :], ALU.subtract), reads=[nlm, laminit], writes=[nlm])

    def emit_norm_mod(li, bi, which, dst, dst_off, f32_dst=None, f32_deps=()):
        t0, w = TBS[bi]
        cond = 1 if bi == 0 else 0
        ab = AB[li]
        x = xT[bi]
        ps = PD.next()
        for dc in range(8):
            sq = sq_p.next()
            S.op("act", lambda e, sq=sq, dc=dc: e.activation(sq[:, 0:w], x[:, dc, :], AF.Square), reads=[x], writes=[sq])
            mm(ps, ps[:, 0:w], cmat, cmat[:, ONES, :], sq, sq[:, 0:w], dc == 0, dc == 7)
        rstd = f32_p.next()
        S.op("act", lambda e: e.activation(rstd[:, 0:w], ps[:, 0:w], AF.Sqrt, bias=EPS * D, scale=1.0), reads=[ps], writes=[rstd])
        S.op("dve", lambda e: e.reciprocal(rstd[:, 0:w], rstd[:, 0:w]), reads=[rstd], writes=[rstd])
        fd = list(f32_deps)
        for dc in range(8):
            tmp = sig_p.next()
            S.op("dve", lambda e, tmp=tmp, dc=dc: e.scalar_tensor_tensor(
                tmp[:, 0:w], x[:, dc, :], ab[:, 3 * which, dc, cond:cond + 1], rstd[:, 0:w], ALU.mult, ALU.mult),
                reads=[x, ab, rstd], writes=[tmp])
            if f32_dst is not None:
                S.op("act", lambda e, tmp=tmp, dc=dc: e.activation(
                    f32_dst[:, dc, 0:w], tmp[:, 0:w], AF.Identity, bias=ab[:, 3 * which + 1, dc, cond:cond + 1]),
                    reads=[tmp, ab], writes=[f32_dst] + fd)
                S.op("pool", lambda e, dc=dc: e.tensor_copy(dst[:, dc, dst_off:dst_off + w], f32_dst[:, dc, 0:w]),
                     reads=[f32_dst] + fd, writes=[dst])
            else:
                S.op("act", lambda e, tmp=tmp, dc=dc: e.activation(
                    dst[:, dc, dst_off:dst_off + w], tmp[:, 0:w], AF.Identity, bias=ab[:, 3 * which + 1, dc, cond:cond + 1]),
                    reads=[tmp, ab], writes=[dst])

    def group_range(g):
        t0 = TBS[GROUPS[g][0]][0]
        t1 = TBS[GROUPS[g][-1]][0] + TBS[GROUPS[g][-1]][1]
        return t0, t1

    def emit_phase_P(li, g):
        phase_start()
        bf_p = Pool([carve([128, 512], BF16, dma=True) for _ in range(3)])
        st_p = Pool([carve([128, 512], BF16, dma=True) for _ in range(3)])
        hT = carve([128, 8, 1280], BF16)
        cosA = carve([128, 1024], F32, dma=True); sinA = carve([128, 1024], F32, dma=True)
        cosB = carve([128, 1024], F32, dma=True); sinB = carve([128, 1024], F32, dma=True)
        cqn = carve([128, 2, 1280], BF16)
        ckvn = carve([128, 1280], BF16)
        wuq = carve([128, 2, 768], BF16, dma=True)
        wukv = carve([128, 1024], BF16, dma=True)
        gg = gains[li]

        gt0, gt1 = group_range(g)
        blocks = GROUPS[g]
        lat0 = max(gt0, NCTX) - NCTX
        latn = gt1 - NCTX - lat0
        for tb_, src in ((cosA, cosA_d), (sinA, sinA_d), (cosB, cosB_d), (sinB, sinB_d)):
            dma_in(tb_, tb_[:, 0:latn], src[:, lat0:lat0 + latn])
        dma_in(wuq, wuq[:, :, :], wuq_d[li].rearrange("(c p) n -> p c n", p=128), q="pool")
        dma_in(wukv, wukv[:, :], wukv_d[li], q="pool")
        for bi in blocks:
            emit_norm_mod(li, bi, 0, hT, TBS[bi][0] - gt0)
        w_l = winp_d[li]
        ropeA = (R64, cosA, sinA)
        ropeB = (R32, cosB, sinB)

        def fm_finish(raw_ps, rows, w, blk, dim, gcol, rope, lat_off, dsts, is_ctx):
            sq = sq_p.next()
            S.op("act", lambda e: e.activation(sq[0:rows, 0:w], raw_ps[0:rows, 0:w], AF.Square), reads=[raw_ps], writes=[sq])
            ss = PB.next()
            mm(ss, ss[0:rows, 0:w], cmat, cmat[0:rows, blk, 0:rows], sq, sq[0:rows, 0:w], True, True)
            rstd = f32_p.next()
            S.op("act", lambda e: e.activation(rstd[0:rows, 0:w], ss[0:rows, 0:w], AF.Sqrt, bias=EPS * dim, scale=1.0), reads=[ss], writes=[rstd])
            S.op("dve", lambda e: e.reciprocal(rstd[0:rows, 0:w], rstd[0:rows, 0:w]), reads=[rstd], writes=[rstd])
            qn = bf_p.next()
            S.op("dve", lambda e: e.scalar_tensor_tensor(qn[0:rows, 0:w], raw_ps[0:rows, 0:w], gg[0:rows, gcol:gcol + 1],
                                                        rstd[0:rows, 0:w], ALU.mult, ALU.mult),
                 reads=[raw_ps, gg, rstd], writes=[qn])
            if rope is not None and not is_ctx:
                rmat, cs, sn = rope
                rot = PC.next()
                mm(rot, rot[0:rows, 0:w], cmat, cmat[0:rows, rmat, 0:rows], qn, qn[0:rows, 0:w], True, True)
                t1 = f32_p.next()
                S.op("pool", lambda e: e.tensor_tensor(t1[0:rows, 0:w], qn[0:rows, 0:w], cs[0:rows, lat_off:lat_off + w], ALU.mult),
                     reads=[qn, cs], writes=[t1])
                t2 = f32_p.next()
                S.op("dve", lambda e: e.tensor_tensor(t2[0:rows, 0:w], rot[0:rows, 0:w], sn[0:rows, lat_off:lat_off + w], ALU.mult),
                     reads=[rot, sn], writes=[t2])
                ob = st_p.next()
                S.op("pool", lambda e: e.tensor_tensor(ob[0:rows, 0:w], t1[0:rows, 0:w], t2[0:rows, 0:w], ALU.add),
                     reads=[t1, t2], writes=[ob])
            else:
                ob = qn
            for (r0, r1, dbuf, dap) in dsts:
                dma_out(dbuf, dap, ob, ob[r0:r1, 0:w])

        def kdst(bi, r0, nrows, krow0):
            t0, w = TBS[bi]
            if bi == 0:
                return (r0, r0 + nrows, ktc_b, ktc_d[krow0:krow0 + nrows, 0:w])
            return (r0, r0 + nrows, kvloc_b, kv_loc[krow0:krow0 + nrows, t0 - NCTX:t0 - NCTX + w])

        def qdst(bi, r0, nrows, qrow0):
            t0, w = TBS[bi]
            return (r0, r0 + nrows, qt_b, qt_d[qrow0:qrow0 + nrows, t0:t0 + w])

        for rc0 in range(0, 17, 4):
            nrc = min(4, 17 - rc0)
            wb = wchunk(w_l, rc0 * 128, nrc * 128)
            for bi in blocks:
                t0, w = TBS[bi]
                ho = t0 - gt0
                lo = t0 - NCTX - lat0
                ctxb = (bi == 0)
                raw5 = None
                for r in range(nrc):
                    idx = rc0 + r
                    raw = PA.next()
                    for dc in range(8):
                        mm(raw, raw[:, 0:w], wb, wb[:, dc, r * 128:(r + 1) * 128], hT, hT[:, dc, ho:ho + w], dc == 0, dc == 7)
                    if idx < 4:
                        fm_finish(raw, 128, w, BLK64, 64, 16, ropeA, lo, [qdst(bi, 0, 128, idx * 128)], ctxb)
                    elif idx == 4:
                        fm_finish(raw, 128, w, BLK64, 64, 17, ropeA, lo, [kdst(bi, 0, 128, 0)], ctxb)
                    elif idx == 5:
                        raw5 = raw
                    elif idx == 6:
                        ss = PB.next()
                        for k, rp in enumerate((raw5, raw)):
                            sq = sq_p.next()
                            S.op("act", lambda e, sq=sq, rp=rp, w=w: e.activation(sq[:, 0:w], rp[:, 0:w], AF.Square), reads=[rp], writes=[sq])
                            mm(ss, ss[:, 0:w], cmat, cmat[:, ONES, :], sq, sq[:, 0:w], k == 0, k == 1)
                        rstd = f32_p.next()
                        S.op("act", lambda e, rstd=rstd, ss=ss, w=w: e.activation(rstd[:, 0:w], ss[:, 0:w], AF.Sqrt, bias=EPS * 256, scale=1.0), reads=[ss], writes=[rstd])
                        S.op("dve", lambda e, rstd=rstd, ss=ss, w=w: e.reciprocal(rstd[:, 0:w], rstd[:, 0:w]), reads=[rstd], writes=[rstd])
                        for k, rp in enumerate((raw5, raw)):
                            S.op("dve", lambda e, k=k, rp=rp, rstd=rstd, w=w, ho=ho: e.scalar_tensor_tensor(
                                cqn[:, k, ho:ho + w], rp[:, 0:w], gg[:, 18 + k:19 + k], rstd[:, 0:w], ALU.mult, ALU.mult),
                                reads=[rp, gg, rstd], writes=[cqn])
                    elif idx == 7:
                        sq = sq_p.next()
                        S.op("act", lambda e, sq=sq, raw=raw, w=w: e.activation(sq[:, 0:w], raw[:, 0:w], AF.Square), reads=[raw], writes=[sq])
                        ss = PB.next()
                        mm(ss, ss[:, 0:w], cmat, cmat[:, ONES, :], sq, sq[:, 0:w], True, True)
                        rstd = f32_p.next()
                        S.op("act", lambda e, rstd=rstd, ss=ss, w=w: e.activation(rstd[:, 0:w], ss[:, 0:w], AF.Sqrt, bias=EPS * 128, scale=1.0), reads=[ss], writes=[rstd])
                        S.op("dve", lambda e, rstd=rstd, ss=ss, w=w: e.reciprocal(rstd[:, 0:w], rstd[:, 0:w]), reads=[rstd], writes=[rstd])
                        S.op("dve", lambda e, raw=raw, rstd=rstd, w=w, ho=ho: e.scalar_tensor_tensor(
                            ckvn[:, ho:ho + w], raw[:, 0:w], gg[:, 20:21], rstd[:, 0:w], ALU.mult, ALU.mult),
                            reads=[raw, gg, rstd], writes=[ckvn])
                    elif idx < 12:
                        fm_finish(raw, 128, w, BLK32, 32, 25, ropeB, lo, [qdst(bi, 0, 128, 1280 + (idx - 8) * 128)], ctxb)
                    elif idx < 16:
                        fm_finish(raw, 128, w, BLK32, 32, 26, ropeB, lo, [kdst(bi, 0, 128, 672 + (idx - 12) * 128)], ctxb)
                    else:
                        fm_finish(raw, 32, w, BLK32, 32, 24, ropeB, lo, [kdst(bi, 0, 32, 640)], ctxb)
        wv1 = wchunk(w_l, FMW, 512)
        wv2 = wchunk(w_l, FMW + 512, 128)
        vreg = kv_loc[KT_ROWS:KV_ROWS, :].rearrange("r c -> (r c)").rearrange("(t v) -> t v", v=V_COLS)

        def vdst(bi, s_, c0, ncol):
            t0, w = TBS[bi]
            if bi == 0:
                return (vc_b, vc_d[s_ * 128:(s_ + 1) * 128, c0:c0 + ncol])
            tl = t0 - NCTX + s_ * 128
            return (kvloc_b, vreg[tl:tl + 128, c0:c0 + ncol])

        def tm_chunk(bi, s_, lhs_buf, lhs_aps, rhs_buf, rhs_aps, ncol, vcol0):
            ps = PA.next()
            nk = len(lhs_aps)
            for k in range(nk):
                mm(ps, ps[:, 0:ncol], lhs_buf, lhs_aps[k], rhs_buf, rhs_aps[k], k == 0, k == nk - 1)
            ob = st_p.next()
            S.op("act", lambda e: e.copy(ob[:, 0:ncol], ps[:, 0:ncol]), reads=[ps], writes=[ob])
            dbuf, dap = vdst(bi, s_, vcol0, ncol)
            dma_out(dbuf, dap, ob, ob[:, 0:ncol])

        for bi in blocks:
            t0, w = TBS[bi]
            ho = t0 - gt0
            for s_ in range(w // 128):
                hs = [hT[:, k, ho + s_ * 128:ho + (s_ + 1) * 128] for k in range(8)]
                tm_chunk(bi, s_, hT, hs, wv1, [wv1[:, k, 0:128] for k in range(8)], 128, 0)
                tm_chunk(bi, s_, hT, hs, wv1, [wv1[:, k, 128:512] for k in range(8)], 384, 640)
                tm_chunk(bi, s_, hT, hs, wv2, [wv2[:, k, 0:128] for k in range(8)], 128, 640 + 384)
        for bi in blocks:
            t0, w = TBS[bi]
            ho = t0 - gt0
            lo = t0 - NCTX - lat0
            ctxb = (bi == 0)
            for i in range(4):
                raw = PA.next()
                for k in range(2):
                    mm(raw, raw[:, 0:w], wuq, wuq[:, k, i * 128:(i + 1) * 128], cqn, cqn[:, k, ho:ho + w], k == 0, k == 1)
                fm_finish(raw, 128, w, BLK64, 64, 21, None, lo,
                          [qdst(bi, 0, 64, 512 + (2 * i) * 96), qdst(bi, 64, 64, 512 + (2 * i + 1) * 96)], ctxb)
            for i in range(2):
                raw = PA.next()
                for k in range(2):
                    mm(raw, raw[:, 0:w], wuq, wuq[:, k, 512 + i * 128:512 + (i + 1) * 128], cqn, cqn[:, k, ho:ho + w], k == 0, k == 1)
                fm_finish(raw, 128, w, BLK32, 32, 22, ropeB, lo,
                          [qdst(bi, 32 * j, 32, 512 + (4 * i + j) * 96 + 64) for j in range(4)], ctxb)
            for i in range(4):
                raw = PA.next()
                mm(raw, raw[:, 0:w], wukv, wukv[:, i * 128:(i + 1) * 128], ckvn, ckvn[:, ho:ho + w], True, True)
                fm_finish(raw, 128, w, BLK64, 64, 23, None, lo, [kdst(bi, 0, 128, 128 + i * 128)], ctxb)
            for s_ in range(w // 128):
                tm_chunk(bi, s_, ckvn, [ckvn[:, ho + s_ * 128:ho + (s_ + 1) * 128]], wukv, [wukv[:, 512:1024]], 512, 128)
        if dbg and dbg.get("dumpP") and g == 0:
            S.barrier()
            dsm = S.new_sem(16)
            for nm, bf_, shp in (("d_cqn", cqn, [128, 2, 1280]), ("d_ckvn", ckvn, [128, 1280]), ("d_wuq", wuq, [128, 2, 768]),
                                 ("d_wukv", wukv, [128, 1024]), ("d_hT", hT, [128, 8, 1280])):
                dd = nc.dram_tensor(nm, shp, BF16, kind="ExternalOutput").ap()
                idx_ = tuple(slice(None) for _ in shp)
                S.dma("sp", dd[idx_], bf_[idx_], dsm, reads=[bf_], writes=[])
            print("sbuf remaining", nc.sbuf_bytes_remaining)

    def head_table():
        hs = []
        for h in range(8):
            hs.append(dict(kind="A", krows=[((h // 4) * 64, 64)], vcol=(h // 4) * 64, qrows=[(h * 64, 64)],
                           scale=64 ** -0.5, orow=h * 64, maps=[(0, 64)]))
        for h in range(8):
            hs.append(dict(kind="B", krows=[(128 + h * 64, 64), (640, 32)], vcol=128 + h * 64,
                           qrows=[(512 + h * 96, 96)], scale=96 ** -0.5, orow=512 + h * 64, maps=[(0, 96)]))
        for h in range(8):
            hs.append(dict(kind="C", krows=[(672 + 2 * h * 32, 64)], vcol=640 + h * 64,
                           qrows=[(1280 + 2 * h * 32, 64)], scale=32 ** -0.5, orow=1024 + h * 64, maps=[(0, 32), (32, 32)]))
        return hs

    def emit_attention(li, last, heads=None):
        phase_start()
        ktb = [Buf(RING[0:96, j * 8192:(j + 1) * 8192], psem()) for j in range(2)]
        kcb = [carve([96, NCTX], BF16, dma=True) for _ in range(2)]
        vtb = [carve([128, NKT, 65], BF16, dma=True) for _ in range(2)]
        qtb = [carve([96, NT], BF16, dma=True) for _ in range(2)]
        pt_p = Pool([carve([128, 512], BF16) for _ in range(4)])
        osb_p = Pool([carve([128, 512], F32) for _ in range(2)])
        rl_p = Pool([carve([128, 512], F32) for _ in range(2)])
        o_p = Pool([carve([64, 512], F32) for _ in range(4)])
        on_p = Pool([carve([64, 512], BF16, dma=True) for _ in range(3)])
        gg = gains[li]
        nlm = neglam[li]
        hs = head_table()
        if heads is not None:
            hs = [hs[i] for i in heads]
        for vb in vtb:
            S.op("pool", lambda e, vb=vb: e.memset(vb[:, :, 64:65], 1.0), reads=[], writes=[vb])
        vreg_all = [kv_all[r * KV_ROWS + KT_ROWS:(r + 1) * KV_ROWS, :].rearrange("r c -> (r c)").rearrange("(t v) -> t v", v=V_COLS)
                    for r in range(4)]
        for hi, hd in enumerate(hs):
            kb = ktb[hi % 2]; kc = kcb[hi % 2]; vb = vtb[hi % 2]; qb = qtb[hi % 2]
            r_off = 0
            for (kr0, nr) in hd["krows"]:
                S.dma("sp", kc[r_off:r_off + nr, :], ktc_d[kr0:kr0 + nr, :], kc.dsem, reads=[ktc_b], writes=[kc])
                for r in range(4):
                    S.dma("sp", kb[r_off:r_off + nr, r * NLAT:(r + 1) * NLAT],
                          kv_all[r * KV_ROWS + kr0:r * KV_ROWS + kr0 + nr, :], kb.dsem, reads=[kvall_b], writes=[kb])
                r_off += nr
            vc0 = hd["vcol"]
            S.dma("sp", vb[:, 0:2, 0:64], vc_d[:, vc0:vc0 + 64].rearrange("(k p) c -> p k c", p=128), vb.dsem,
                  reads=[vc_b], writes=[vb])
            for r in range(4):
                S.dma("sp", vb[:, 2 + 16 * r:2 + 16 * (r + 1), 0:64],
                      vreg_all[r][:, vc0:vc0 + 64].rearrange("(k p) c -> p k c", p=128), vb.dsem,
                      reads=[kvall_b], writes=[vb])
            r_off = 0
            for (qr0, nr) in hd["qrows"]:
                S.dma("sp", qb[r_off:r_off + nr, :], qt_d[qr0:qr0 + nr, :], qb.dsem, reads=[qt_b], writes=[qb])
                r_off += nr
            orow = hd["orow"]
            for bi, (t0, w) in enumerate(TBS):
                if bi == 0 and last:
                    continue
                nkt = 2 if bi == 0 else NKT
                onorm = []
                for (mr0, dk) in hd["maps"]:
                    ops = PB.next()
                    spsl = {}

                    def issue_S(kt):
                        sps_ = PA.next()
                        if kt < 2:
                            mm(sps_, sps_[:, 0:w], kc, kc[mr0:mr0 + dk, kt * 128:(kt + 1) * 128], qb, qb[mr0:mr0 + dk, t0:t0 + w], True, True)
                        else:
                            mm(sps_, sps_[:, 0:w], kb, kb[mr0:mr0 + dk, (kt - 2) * 128:(kt - 1) * 128], qb, qb[mr0:mr0 + dk, t0:t0 + w], True, True)
                        spsl[kt] = sps_

                    LA = 2
                    for kt in range(min(LA, nkt)):
                        issue_S(kt)
                    for kt in range(nkt):
                        if kt + LA < nkt:
                            issue_S(kt + LA)
                        sps = spsl.pop(kt)
                        pt = pt_p.next()
                        S.op("act", lambda e, pt=pt, sps=sps, w=w, sc=hd["scale"]: e.activation(pt[:, 0:w], sps[:, 0:w], AF.Exp, scale=sc),
                             reads=[sps], writes=[pt])
                        mm(ops, ops[0:65, 0:w], vb, vb[:, kt, 0:65], pt, pt[:, 0:w], kt == 0, kt == nkt - 1)
                    rl = rl_p.next()
                    S.op("dve", lambda e, rl=rl, ops=ops, w=w: e.reciprocal(rl[64:65, 0:w], ops[64:65, 0:w]), reads=[ops], writes=[rl])
                    osb = osb_p.next()
                    S.op("act", lambda e, osb=osb, ops=ops, w=w: e.copy(osb[0:64, 0:w], ops[0:64, 0:w]), reads=[ops], writes=[osb])
                    bc = PC.next()
                    mm(bc, bc[0:64, 0:w], c32, c32[64:65, 0, 0:64], rl, rl[64:65, 0:w], True, True)
                    if hd["kind"] == "C":
                        o_ = o_p.next()
                    else:
                        o_ = on_p.next()
                    S.op("dve", lambda e, o_=o_, osb=osb, bc=bc, w=w: e.tensor_tensor(o_[:, 0:w], osb[0:64, 0:w], bc[0:64, 0:w], ALU.mult),
                         reads=[osb, bc], writes=[o_])
                    onorm.append(o_)
                if hd["kind"] == "C":
                    o1b, o2 = onorm
                    od = o_p.next()
                    S.op("dve", lambda e, od=od, o2=o2, o1b=o1b, w=w: e.scalar_tensor_tensor(
                        od[:, 0:w], o2[:, 0:w], nlm[0:64, 0:1], o1b[:, 0:w], ALU.mult, ALU.add),
                        reads=[o2, o1b, nlm], writes=[od])
                    sq = sq_p.next()
                    S.op("act", lambda e, sq=sq, od=od, w=w: e.activation(sq[0:64, 0:w], od[:, 0:w], AF.Square), reads=[od], writes=[sq])
                    ss = PC.next()
                    mm(ss, ss[0:64, 0:w], cmat, cmat[0:64, BLK64, 0:64], sq, sq[0:64, 0:w], True, True)
                    rstd = f32_p.next()
                    S.op("act", lambda e, rstd=rstd, ss=ss, w=w: e.activation(rstd[0:64, 0:w], ss[0:64, 0:w], AF.Sqrt, bias=EPS * 64, scale=1.0), reads=[ss], writes=[rstd])
                    S.op("dve", lambda e, rstd=rstd, ss=ss, w=w: e.reciprocal(rstd[0:64, 0:w], rstd[0:64, 0:w]), reads=[rstd], writes=[rstd])
                    on = on_p.next()
                    S.op("dve", lambda e, on=on, od=od, rstd=rstd, w=w: e.scalar_tensor_tensor(
                        on[:, 0:w], od[:, 0:w], gg[0:64, 27:28], rstd[0:64, 0:w], ALU.mult, ALU.mult),
                        reads=[od, gg, rstd], writes=[on])
                else:
                    on = onorm[0]
                dma_out(ot_b, ot_d[orow:orow + 64, t0:t0 + w], on, on[:, 0:w])

    def emit_merge(li, g, last):
        phase_start()
        hT = carve([128, 8, 1280], BF16)
        otg = Pool([carve([128, 4, 1280], BF16, dma=True) for _ in range(2)])
        yT = carve([128, 8, 1280], BF16)
        wo_p = Pool([carve([128, 4, 512], BF16, dma=True) for _ in range(2)])
        gt0, gt1 = group_range(g)
        blocks = [b for b in GROUPS[g] if not (last and b == 0)]
        ab = AB[li]
        for bi in blocks:
            emit_norm_mod(li, bi, 0, hT, TBS[bi][0] - gt0)
        wos = [woa_d[li], wob_d[li], woc_d[li]]
        for m in range(3):
            og = otg.next()
            dma_in(og, og[:, :, 0:gt1 - gt0],
                   ot_d[m * 512:(m + 1) * 512, gt0:gt1].rearrange("(c p) t -> p c t", p=128), extra_reads=[ot_b])
            for jg in range(2):
                gw = wchunk(winp_d[li], FMW + TMW + m * 1024 + jg * 512, 512)
                ow = wo_p.next()
                dma_in(ow, ow[:, :, :], wos[m][:, jg * 512:(jg + 1) * 512].rearrange("(c p) n -> p c n", p=128), q="pool")
                for bi in blocks:
                    t0, w = TBS[bi]
                    ho = t0 - gt0
                    for j4 in range(4):
                        j = jg * 4 + j4
                        pp = PA.next()
                        for kc_ in range(4):
                            mm(pp, pp[:, 0:w], ow, ow[:, kc_, j4 * 128:(j4 + 1) * 128], og, og[:, kc_, ho:ho + w], kc_ == 0, kc_ == 3)
                        gp = PB.next()
                        for dc in range(8):
                            mm(gp, gp[:, 0:w], gw, gw[:, dc, j4 * 128:(j4 + 1) * 128], hT, hT[:, dc, ho:ho + w], dc == 0, dc == 7)
                        sg = sig_p.next()
                        S.op("act", lambda e, sg=sg, gp=gp, w=w: e.activation(sg[:, 0:w], gp[:, 0:w], AF.Sigmoid), reads=[gp], writes=[sg])
                        if m == 0:
                            S.op("dve", lambda e, sg=sg, pp=pp, j=j, ho=ho, w=w: e.tensor_tensor(yT[:, j, ho:ho + w], pp[:, 0:w], sg[:, 0:w], ALU.mult),
                                 reads=[pp, sg], writes=[yT])
                        else:
                            tmp = f32_p.next()
                            S.op("dve", lambda e, tmp=tmp, sg=sg, pp=pp, w=w: e.tensor_tensor(tmp[:, 0:w], pp[:, 0:w], sg[:, 0:w], ALU.mult),
                                 reads=[pp, sg], writes=[tmp])
                            S.op("pool", lambda e, tmp=tmp, j=j, ho=ho, w=w: e.tensor_tensor(yT[:, j, ho:ho + w], yT[:, j, ho:ho + w], tmp[:, 0:w], ALU.add),
                                 reads=[tmp, yT], writes=[yT])
        for jg in range(2):
            ww = wchunk(wout_d[li], jg * 512, 512)
            for bi in blocks:
                t0, w = TBS[bi]
                ho = t0 - gt0
                cond = 1 if bi == 0 else 0
                for j4 in range(4):
                    j = jg * 4 + j4
                    zp = PA.next()
                    for dc in range(8):
                        mm(zp, zp[:, 0:w], ww, ww[:, dc, j4 * 128:(j4 + 1) * 128], yT, yT[:, dc, ho:ho + w], dc == 0, dc == 7)
                    x = xT[bi]
                    S.op("dve", lambda e, zp=zp, x=x, j=j, w=w, cond=cond: e.scalar_tensor_tensor(
                        x[:, j, :], zp[:, 0:w], ab[:, 2, j, cond:cond + 1], x[:, j, :], ALU.mult, ALU.add),
                        reads=[zp, ab, x], writes=[x])

    def emit_moe(li, last, n_exp=32):
        phase_start()
        h2 = [carve([128, 8, w], BF16) for (_, w) in TBS]
        o_act = uoff[0]
        act_all = carve([128, 4, NT], BF16)
        actT = [Buf(act_all[:, :, t0:t0 + w]) for (t0, w) in TBS]
        h2f = Buf(UF[:, o_act // 2:o_act // 2 + 4096].rearrange("p (c t) -> p c t", t=512))
        GT = carve([32, NT], BF16)
        GTm = Pool([carve([32, 512], BF16) for _ in range(2)])
        rw = carve([128, 8, 32], F32, dma=True)
        rb = carve([128, 32], F32, dma=True)
        bgu = carve([128, 32, 16], F32, dma=True)
        bgu1 = carve([128, 32, 8], F32)
        bdn = carve([32, 1024], BF16, dma=True)
        rt = carve([128, 4, 48], F32)
        gbc_p = Pool([carve([128, 512], BF16) for _ in range(2)])
        blocks = [b for b in range(5) if not (last and b == 0)]
        ab = AB[li]
        dma_in(rw, rw[:, :, :], rw_d[li])
        dma_in(rb, rb[:, :], rb_d[li])
        dma_in(bgu, bgu[:, :, :], bgu_d[li])
        dma_in(bdn, bdn[:, :], bdn_d[li], q="pool")
        S.op("dve", lambda e: e.tensor_scalar(bgu1[:, :, :], bgu[:, :, 8:16], 1.0, None, ALU.add), reads=[bgu], writes=[bgu1])
        for bi in blocks:
            t0, w = TBS[bi]
            emit_norm_mod(li, bi, 1, h2[bi], 0, f32_dst=h2f)
            for s_ in range(w // 128):
                lp = PC.next()
                for dc in range(8):
                    mm(lp, lp[:, 0:32], h2f, h2f[:, dc, s_ * 128:(s_ + 1) * 128], rw, rw[:, dc, :], dc == 0, dc == 7)
                r = rt
                S.op("dve", lambda e, lp=lp: e.tensor_tensor(r[:, 0, 0:32], lp[:, 0:32], rb[:, :], ALU.add), reads=[lp, rb], writes=[r])
                S.op("dve", lambda e: e.max(r[:, 1, 0:8], r[:, 0, 0:32]), reads=[r], writes=[r])
                S.op("dve", lambda e: e.tensor_scalar(r[:, 1, 8:9], r[:, 1, 0:1], -1.0, None, ALU.mult), reads=[r], writes=[r])
                S.op("dve", lambda e: e.tensor_scalar(r[:, 2, 0:32], r[:, 0, 0:32], r[:, 1, 3:4], None, ALU.is_ge), reads=[r], writes=[r])
                S.op("act", lambda e: e.activation(r[:, 3, 0:32], r[:, 0, 0:32], AF.Exp, bias=r[:, 1, 8:9]), reads=[r], writes=[r])
                S.op("dve", lambda e: e.tensor_tensor(r[:, 3, 0:32], r[:, 3, 0:32], r[:, 2, 0:32], ALU.mult), reads=[r], writes=[r])
                S.op("dve", lambda e: e.reduce_sum(r[:, 1, 9:10], r[:, 3, 0:32], AX.X), reads=[r], writes=[r])
                S.op("dve", lambda e: e.reciprocal(r[:, 1, 10:11], r[:, 1, 9:10]), reads=[r], writes=[r])
                S.op("dve", lambda e: e.tensor_scalar(r[:, 3, 0:32], r[:, 3, 0:32], r[:, 1, 10:11], None, ALU.mult), reads=[r], writes=[r])
                tp = PC.next()
                S.op("pe", lambda e, tp=tp: e.transpose(tp[0:32, 0:128], r[:, 3, 0:32], c32[:, 1, :]), reads=[r, c32], writes=[tp])
                S.op("act", lambda e, tp=tp, t0=t0, s_=s_: e.copy(GT[:, t0 + s_ * 128:t0 + (s_ + 1) * 128], tp[0:32, 0:128]), reads=[tp], writes=[GT])
        S.barrier()
        if dbg and dbg.get("dumpM"):
            dsm = S.new_sem(16)
            dd = nc.dram_tensor("d_GT", [32, NT], BF16, kind="ExternalOutput").ap()
            S.dma("sp", dd[:, :], GT[:, :], dsm, reads=[GT], writes=[])
            for bi_ in range(5):
                dd = nc.dram_tensor("d_h2_%d" % bi_, [128, 8, TBS[bi_][1]], BF16, kind="ExternalOutput").ap()
                S.dma("sp", dd[:, :, :], h2[bi_][:, :, :], dsm, reads=[h2[bi_]], writes=[])
            S.barrier()
        for bi in blocks:
            t0, w = TBS[bi]
            cond = 1 if bi == 0 else 0
            x = xT[bi]
            for j in range(8):
                bp = PC.next()
                mm(bp, bp[:, 0:w], bdn, bdn[:, j * 128:(j + 1) * 128], GT, GT[:, t0:t0 + w], True, True)
                S.op("dve", lambda e, bp=bp, x=x, j=j, w=w, cond=cond: e.scalar_tensor_tensor(
                    x[:, j, :], bp[:, 0:w], ab[:, 5, j, cond:cond + 1], x[:, j, :], ALU.mult, ALU.add),
                    reads=[bp, ab, x], writes=[x])
        pgP = Pool(banks[0:3])
        plP = Pool([banks[3], banks[4], banks[7]])
        units = [(ex_, half_) for ex_ in range(n_exp) for half_ in range(2)]

        def load_gl(u_):
            ex_, half_ = units[u_]
            return (wchunk(wgu_d[li, ex_], half_ * 512, 512), wchunk(wgu_d[li, ex_], 1024 + half_ * 512, 512))

        pre = {0: load_gl(0)}
        for u_, (ex, half) in enumerate(units):
            if True:
                wd = wdn_d[li, ex]
                gl, ln = pre.pop(u_)
                dwb = ring.next()
                dwv = RING_view(dwb)
                S.dma("pool", dwv, wd[half * 512:(half + 1) * 512, :].rearrange("(c p) n -> p c n", p=128), dwb.dsem, reads=[], writes=[dwb])
                if u_ + 1 < len(units):
                    pre[u_ + 1] = load_gl(u_ + 1)
                pending = None
                for bi in blocks:
                    t0, w = TBS[bi]
                    gm_ = GTm.next()
                    S.op("dve", lambda e, gm_=gm_, t0=t0, w=w, ex=ex: e.tensor_scalar(
                        gm_[:, 0:w], GT[:, t0:t0 + w], c32[0:32, 1, ex:ex + 1], None, ALU.mult), reads=[GT, c32], writes=[gm_])
                    gps = PC.next()
                    mm(gps, gps[:, 0:w], cmat, cmat[0:32, ONES, :], gm_, gm_[:, 0:w], True, True)
                    gb = gbc_p.next()
                    S.op("act", lambda e, gb=gb, gps=gps, w=w: e.copy(gb[:, 0:w], gps[:, 0:w]), reads=[gps], writes=[gb])
                    for f4 in range(4):
                        fc = half * 4 + f4
                        pg = pgP.next()
                        pl = plP.next()
                        for dc in range(8):
                            mm(pg, pg[:, 0:w], gl, gl[:, dc, f4 * 128:(f4 + 1) * 128], h2[bi], h2[bi][:, dc, :], dc == 0, dc == 7)
                        for dc in range(8):
                            mm(pl, pl[:, 0:w], ln, ln[:, dc, f4 * 128:(f4 + 1) * 128], h2[bi], h2[bi][:, dc, :], dc == 0, dc == 7)
                        gv = f32_p.next()
                        S.op("dve", lambda e, gv=gv, pg=pg, fc=fc, w=w, ex=ex: e.tensor_scalar(
                            gv[:, 0:w], pg[:, 0:w], bgu[:, ex, fc:fc + 1], 7.0, ALU.add, ALU.min), reads=[pg, bgu], writes=[gv])
                        sg = sig_p.next()
                        S.op("act", lambda e, sg=sg, gv=gv, w=w: e.activation(sg[:, 0:w], gv[:, 0:w], AF.Sigmoid, scale=1.702),
                             reads=[gv], writes=[sg])
                        lv = f32_p.next()
                        S.op("dve", lambda e, lv=lv, pl=pl, fc=fc, w=w, ex=ex: e.tensor_scalar(
                            lv[:, 0:w], pl[:, 0:w], bgu1[:, ex, fc:fc + 1], -6.0, ALU.add, ALU.max), reads=[pl, bgu1], writes=[lv])
                        S.op("pool", lambda e, gv=gv, sg=sg, w=w: e.tensor_tensor(gv[:, 0:w], gv[:, 0:w], sg[:, 0:w], ALU.mult),
                             reads=[gv, sg], writes=[gv])
                        S.op("pool", lambda e, gv=gv, gb=gb, w=w: e.tensor_tensor(gv[:, 0:w], gv[:, 0:w], gb[:, 0:w], ALU.mult),
                             reads=[gv, gb], writes=[gv])
                        at = actT[bi]
                        if pending is not None:
                            pending()

                        def pending(at=at, gv=gv, lv=lv, f4=f4, w=w):
                            S.op("dve", lambda e: e.scalar_tensor_tensor(
                                at[:, f4, :], lv[:, 0:w], 8.0, gv[:, 0:w], ALU.min, ALU.mult),
                                reads=[gv, lv], writes=[at])
                if pending is not None:
                    pending()
                    pending = None
                for bi in blocks:
                    t0, w = TBS[bi]
                    cond = 1 if bi == 0 else 0
                    x = xT[bi]
                    at = actT[bi]
                    for j in range(8):
                        yp = pgP.next()
                        for f4 in range(4):
                            mm(yp, yp[:, 0:w], dwb, dwv[:, f4, j * 128:(j + 1) * 128], at, at[:, f4, :], f4 == 0, f4 == 3)
                        S.op("dve", lambda e, yp=yp, x=x, j=j, w=w, cond=cond: e.scalar_tensor_tensor(
                            x[:, j, :], yp[:, 0:w], ab[:, 5, j, cond:cond + 1], x[:, j, :], ALU.mult, ALU.add),
                            reads=[yp, ab, x], writes=[x])

    def RING_view(b):
        return b.ap.rearrange("p c n -> p (c n)").rearrange("p (c n) -> p c n", n=1024)

    def emit_exchange():
        if mode != "fused":
            return
        E = S.E["pool"]
        waits = S._waits(E, [kvloc_b], [kvall_b])
        ccs = S.new_sem(16)
        ccs.n += 1
        E.prog.append((waits, lambda e: e.collective_compute(
            "AllGather", ALU.bypass, [[0, 1, 2, 3], [4, 5, 6, 7]],
            [kv_loc[:, :]], [kv_all[:, :]]), None, (ccs, 1)))
        kvloc_b.r[ccs] = 1
        kvall_b.w = (ccs, 1)
        kvall_b.r = {}

    stop = dbg.get("stop") if dbg else None
    for li in range(nl):
        emit_modulation(li)
    for li in range(nl):
        last = (layers[li] == L - 1)
        emit_phase_P(li, 0)
        emit_phase_P(li, 1)
        if mode == "A" or stop == "P":
            break
        emit_exchange()
        emit_attention(li, last, heads=dbg.get("heads") if dbg else None)
        if stop == "attn":
            break
        emit_merge(li, 0, last)
        emit_merge(li, 1, last)
        if stop == "merge":
            break
        emit_moe(li, last, n_exp=dbg.get("n_exp", 32) if dbg else 32)
    S.barrier()
    if mode == "A":
        S.finish("sp", [kvloc_b])
    else:
        outb = Buf(out_d)
        osem = S.new_sem(16)
        for bi, (t0, w) in enumerate(TBS):
            S.dma("sp", out_d[:, :, t0:t0 + w], xT[bi][:, :, :], osem, reads=[xT[bi]], writes=[outb])
        S.finish("sp", [outb])
    S.emit()
    return nc


def _rope_tables(rot_dim, n_lat):
    n_rows = n_lat // 64
    row = np.repeat(np.arange(n_rows, dtype=np.float32), 64)
    col = np.tile(np.arange(64, dtype=np.float32), n_rows)
    axis_pairs = rot_dim // 4
    inv = (10000.0 ** (-np.arange(axis_pairs, dtype=np.float32) / axis_pairs)).astype(np.float32)
    ang = np.concatenate([row[:, None] * inv, col[:, None] * inv], axis=-1).astype(np.float32)
    return np.cos(ang).astype(np.float32), np.sin(ang).astype(np.float32)


def _const_mats():
    m = np.zeros((128, 6, 128), np.float32)
    m[:, 0, :] = 1.0
    for i in range(128):
        for j in range(128):
            if i // 64 == j // 64:
                m[i, 1, j] = 1.0
            if i // 32 == j // 32:
                m[i, 2, j] = 1.0
    for (slot, blk) in ((3, 64), (4, 32)):
        hh = blk // 2
        for mm_ in range(128):
            i = mm_ % blk
            base = mm_ - i
            if i < hh:
                m[base + i + hh, slot, mm_] = -1.0
            else:
                m[base + i - hh, slot, mm_] = 1.0
    m[:, 5, :] = np.eye(128, dtype=np.float32)
    return m


def _feat_major(v):
    return np.ascontiguousarray(v.reshape(8, 128).T)


def _prep_shared(inp, layers):
    f = np.float32
    out = {}
    ls = list(layers)
    sel = np.zeros((32, 32, 128), f)
    for e in range(32):
        sel[e, e, :] = 1.0
    out["sel"] = sel
    out["cmat"] = _const_mats()
    gm = np.zeros((len(ls), 128, NG), f)
    gains = np.zeros((len(ls), 128, NG), f)
    for i, l in enumerate(ls):
        lam_init = 0.8 - 0.6 * np.exp(-0.3 * l)
        gm[i, :, 0:16] = 32.0
        gm[i, :, 16] = 8.0; gm[i, :, 17] = 8.0
        gm[i, :, 18:20] = 16.0
        gm[i, :, 20] = np.sqrt(128.0)
        gm[i, :, 21] = 8.0; gm[i, :, 22] = np.sqrt(32.0); gm[i, :, 23] = 8.0; gm[i, :, 24] = np.sqrt(32.0)
        gm[i, :, 25] = np.sqrt(32.0); gm[i, :, 26] = np.sqrt(32.0)
        gm[i, :, 27] = 8.0 * (1.0 - lam_init)
        gains[i, :, 0:8] = _feat_major(inp["norm_mix"][l])
        gains[i, :, 8:16] = _feat_major(inp["norm_ffn"][l])
        gains[i, :, 16] = np.tile(inp["a_q_norm"][l], 2)
        gains[i, :, 17] = np.tile(inp["a_k_norm"][l], 2)
        gains[i, :, 18:20] = inp["b_q_a_norm"][l].reshape(2, 128).T
        gains[i, :, 20] = inp["b_kv_a_norm"][l]
        gains[i, :, 21] = np.tile(inp["b_q_norm"][l][:64], 2)
        gains[i, :, 22] = np.tile(inp["b_q_norm"][l][64:], 4)
        gains[i, :, 23] = np.tile(inp["b_k_norm"][l][:64], 2)
        gains[i, :, 24] = np.tile(inp["b_k_norm"][l][64:], 4)
        gains[i, :, 25] = np.tile(inp["c_q_norm"][l], 4)
        gains[i, :, 26] = np.tile(inp["c_k_norm"][l], 4)
        gains[i, :, 27] = np.tile(inp["c_subln"][l], 2)
    out["gmul"] = gm
    out["gains"] = gains
    out["clam"] = np.ascontiguousarray(np.broadcast_to(inp["c_lambda"][ls].reshape(len(ls), 1, 128), (len(ls), 128, 128))).astype(f)
    li_ = np.array([0.8 - 0.6 * np.exp(-0.3 * l) for l in ls], f)
    out["laminit"] = np.ascontiguousarray(np.broadcast_to(li_.reshape(-1, 1, 1), (len(ls), 128, 1))).astype(f)
    out["bmod"] = np.ascontiguousarray(inp["b_mod"][ls].reshape(len(ls), 48, 128).transpose(0, 2, 1))
    out["rb"] = np.ascontiguousarray(np.broadcast_to(inp["router_b"][ls][:, None, :], (len(ls), 128, 32))).astype(f)
    out["bgu"] = np.ascontiguousarray(inp["exp_b_gu"][ls].reshape(len(ls), 32, 16, 128).transpose(0, 3, 1, 2))
    out["bdn"] = np.ascontiguousarray(inp["exp_b_down"][ls])
    out["rw"] = np.ascontiguousarray(inp["router_w"][ls].reshape(len(ls), 8, 128, 32).transpose(0, 2, 1, 3))
    out["wmod"] = np.ascontiguousarray(inp["w_mod"][ls])
    w_in = inp["w_in"][ls]
    sp = np.cumsum([0, 512, 128, 128, 256, 128, 32, 512, 512, 512, 3072])
    a_q, a_k, a_v, b_cq, b_ckv, b_kr, c_q, c_k, c_v, gates = [w_in[:, :, sp[i]:sp[i + 1]] for i in range(10)]
    pad = np.zeros((len(ls), 1024, 96), f)
    out["winp"] = np.ascontiguousarray(np.concatenate([a_q, a_k, b_cq, b_ckv, c_q, c_k, b_kr, pad, a_v, c_v, gates], axis=2))
    uq = inp["b_w_uq"][ls].reshape(len(ls), 256, 8, 96)
    out["wuq"] = np.ascontiguousarray(np.concatenate([uq[..., :64].reshape(len(ls), 256, 512), uq[..., 64:].reshape(len(ls), 256, 256)], axis=2))
    ukv = inp["b_w_ukv"][ls].reshape(len(ls), 128, 8, 128)
    out["wukv"] = np.ascontiguousarray(np.concatenate([ukv[..., :64].reshape(len(ls), 128, 512), ukv[..., 64:].reshape(len(ls), 128, 512)], axis=2))
    out["woa"] = np.ascontiguousarray(inp["w_o_a"][ls]); out["wob"] = np.ascontiguousarray(inp["w_o_b"][ls]); out["woc"] = np.ascontiguousarray(inp["w_o_c"][ls])
    out["wout"] = np.ascontiguousarray(inp["w_out"][ls])
    out["wgu"] = np.ascontiguousarray(inp["exp_w_gu"][ls])
    out["wdn"] = np.ascontiguousarray(inp["exp_w_down"][ls])
    return out


def _prep_core(inp, core, x_lat, x_ctx):
    b, r = core // 4, core % 4
    f = np.float32
    xt = np.concatenate([x_ctx[b], x_lat[b, r * NLAT:(r + 1) * NLAT]], axis=0)
    xT = np.ascontiguousarray(xt.reshape(NT, 8, 128).transpose(2, 1, 0)).astype(f)
    cond = np.stack([inp["c"][b], inp["c_ctx"]], axis=-1)
    condT = np.ascontiguousarray(cond.reshape(8, 128, 2).transpose(1, 0, 2)).astype(f)
    out = {"xT_in": xT, "condT": condT}
    for nm, rd, blk in (("A", 64, 64), ("B", 32, 32)):
        cs, sn = _rope_tables(rd, 8192)
        cs = cs[r * NLAT:(r + 1) * NLAT]; sn = sn[r * NLAT:(r + 1) * NLAT]
        idx = (np.arange(128) % blk) % (rd // 2)
        out["cos" + nm] = np.ascontiguousarray(cs[:, idx].T).astype(f)
        out["sin" + nm] = np.ascontiguousarray(sn[:, idx].T).astype(f)
    return out


def _unpack_x(res_list):
    x_lat = np.zeros((2, 8192, 1024), np.float32)
    x_ctx = np.zeros((2, 256, 1024), np.float32)
    for core, o in enumerate(res_list):
        b, r = core // 4, core % 4
        xt = np.asarray(o).transpose(2, 1, 0).reshape(NT, 1024)
        x_lat[b, r * NLAT:(r + 1) * NLAT] = xt[NCTX:]
        if r == 0:
            x_ctx[b] = xt[:NCTX]
    return x_lat, x_ctx


A_KEYS = ("xT_in", "condT", "cosA", "sinA", "cosB", "sinB", "cmat", "gmul", "gains", "clam", "laminit", "bmod",
          "wmod", "winp", "wuq", "wukv")


def _filter(m, mode):
    if mode == "A":
        return {k: v for k, v in m.items() if k in A_KEYS}
    return {k: v for k, v in m.items() if k != "sel"}


_CACHE = {}


def _get_prog(mode, last):
    key = (mode, bool(last))
    if key not in _CACHE:
        _CACHE[key] = build_program(mode, [L - 1 if last else 0])
    return _CACHE[key]


def kernel(**inp):
    inp = {k: np.asarray(v) for k, v in inp.items()}
    x_lat = np.ascontiguousarray(inp["x"], dtype=np.float32)
    x_ctx = np.ascontiguousarray(inp["ctx"], dtype=np.float32)
    for l in range(L):
        last = (l == L - 1)
        shared = _prep_shared(inp, [l])
        maps = []
        for core in range(8):
            m = dict(shared)
            m.update(_prep_core(inp, core, x_lat, x_ctx))
            maps.append(m)
        resA = run_bass_kernel_spmd(_get_prog("A", False), [_filter(m, "A") for m in maps], core_ids=list(range(8)))
        kv = [np.asarray(r["kv_loc"]) for r in resA.results]
        for core in range(8):
            b = core // 4
            maps[core]["kv_all"] = np.ascontiguousarray(np.concatenate(kv[4 * b:4 * b + 4], axis=0))
        resB = run_bass_kernel_spmd(_get_prog("B", last), [_filter(m, "B") for m in maps], core_ids=list(range(8)))
        x_lat, x_ctx_new = _unpack_x([r["out"] for r in resB.results])
        if not last:
            x_ctx = x_ctx_new
    return x_lat.astype(np.float32)
```
